# Optimizing a Trainium2 kernel written in Bass

```python
import jax, jax.numpy as jnp
from jax import lax
import numpy as np

D_MODEL = 1024
BATCH = 16
SEQ = 2048
DEPTH = 2

CHUNK = 64
Q_BLOCK = 128
MLA_HEADS = 8
MLA_NOPE_DIM = 64
MLA_ROPE_DIM = 32
MLA_V_DIM = 64
MLA_Q_RANK = 256
MLA_KV_RANK = 128
ROPE_THETA = 10000.0
FOX_HEADS = 8
FOX_HEAD_DIM = 64
MLA_OUT = MLA_HEADS * MLA_V_DIM
FOX_OUT = FOX_HEADS * FOX_HEAD_DIM
MIX_WIDTH = MLA_OUT + FOX_OUT
IN_WIDTH = MLA_Q_RANK + MLA_KV_RANK + MLA_ROPE_DIM + 3 * FOX_OUT + FOX_HEADS
N_GROUPS = 4
EXPERTS_PER_GROUP = 8
N_EXPERTS = N_GROUPS * EXPERTS_PER_GROUP
EXPERT_TOP_K = 2
D_EXPERT = 512
EXPERT_BLOCK = 128
NORM_EPS = 1e-6

kernel_name = "hymba_mla_fox_hiermoe_trunk"


def rmsnorm(x, g):
    xf = x.astype(jnp.float32)
    y = xf * lax.rsqrt(jnp.mean(xf * xf, axis=-1, keepdims=True) + NORM_EPS)
    return (y * g.astype(jnp.float32)).astype(x.dtype)


def rope_tables(positions):
    half = MLA_ROPE_DIM // 2
    inv_freq = ROPE_THETA ** (-jnp.arange(half, dtype=jnp.float32) / half)
    ang = positions.astype(jnp.float32)[..., None] * inv_freq
    return jnp.cos(ang), jnp.sin(ang)


def apply_rope(x, cos, sin):
    half = x.shape[-1] // 2
    xf = x.astype(jnp.float32)
    x1, x2 = xf[..., :half], xf[..., half:]
    return jnp.concatenate([x1 * cos - x2 * sin, x1 * sin + x2 * cos], axis=-1).astype(x.dtype)


def block_sweep_attention(q, k, v, scale, chunk_causal, log_decay_cum=None):
    S = q.shape[2]
    outs = []
    for i in range(S // Q_BLOCK):
        qs, qe = i * Q_BLOCK, (i + 1) * Q_BLOCK
        s = jnp.einsum('bhqd,bhkd->bhqk', q[:, :, qs:qe], k[:, :, :qe],
                       preferred_element_type=jnp.float32) * scale
        t_idx = jnp.arange(qs, qe)[:, None]
        s_idx = jnp.arange(qe)[None, :]
        if chunk_causal:
            allowed = (s_idx // CHUNK) <= (t_idx // CHUNK)
        else:
            allowed = s_idx <= t_idx
        if log_decay_cum is not None:
            s = s + (log_decay_cum[:, :, qs:qe, None] - log_decay_cum[:, :, None, :qe])
        s = jnp.where(allowed, s, -jnp.inf)
        p = jax.nn.softmax(s, axis=-1).astype(v.dtype)
        outs.append(jnp.einsum('bhqk,bhkd->bhqd', p, v[:, :, :qe]))
    return jnp.concatenate(outs, axis=2)


def mla_group(q_lat, kv_lat, k_rope, positions, q_norm, w_uq, kv_norm, w_ukv):
    B, S, _ = q_lat.shape
    q = (rmsnorm(q_lat, q_norm) @ w_uq).reshape(B, S, MLA_HEADS, MLA_NOPE_DIM + MLA_ROPE_DIM)
    q = q.transpose(0, 2, 1, 3)
    kv = (rmsnorm(kv_lat, kv_norm) @ w_ukv).reshape(B, S, MLA_HEADS, MLA_NOPE_DIM + MLA_V_DIM)
    kv = kv.transpose(0, 2, 1, 3)
    k_nope, v = kv[..., :MLA_NOPE_DIM], kv[..., MLA_NOPE_DIM:]
    cos, sin = rope_tables(positions)
    q_pe = apply_rope(q[..., MLA_NOPE_DIM:], cos[:, None], sin[:, None])
    k_pe = apply_rope(k_rope, cos, sin)[:, None]
    q = jnp.concatenate([q[..., :MLA_NOPE_DIM], q_pe], axis=-1)
    k = jnp.concatenate([k_nope, jnp.broadcast_to(k_pe, (B, MLA_HEADS, S, MLA_ROPE_DIM))], axis=-1)
    o = block_sweep_attention(q, k, v, (MLA_NOPE_DIM + MLA_ROPE_DIM) ** -0.5, chunk_causal=True)
    return o.transpose(0, 2, 1, 3).reshape(B, S, MLA_OUT)


def fox_group(q, k, v, f_logit, b_forget):
    B, S, _ = q.shape
    def to_heads(t):
        return t.reshape(B, S, FOX_HEADS, FOX_HEAD_DIM).transpose(0, 2, 1, 3)
    log_f = jax.nn.log_sigmoid(f_logit.astype(jnp.float32) + b_forget.astype(jnp.float32))
    c = jnp.cumsum(log_f, axis=1).transpose(0, 2, 1)
    o = block_sweep_attention(to_heads(q), to_heads(k), to_heads(v), FOX_HEAD_DIM ** -0.5,
                              chunk_causal=False, log_decay_cum=c)
    return o.transpose(0, 2, 1, 3).reshape(B, S, FOX_OUT)


def hier_moe(h, w_rg, b_rg, w_re, b_re, w_gate, w_up, w_down):
    B, S, D = h.shape
    N = B * S
    t = h.reshape(N, D)
    g_prob = jax.nn.softmax((t @ w_rg).astype(jnp.float32) + b_rg.astype(jnp.float32), axis=-1)
    g_val, g_idx = lax.top_k(g_prob, 1)
    e_logits = ((t @ w_re).astype(jnp.float32) + b_re.astype(jnp.float32)).reshape(N, N_GROUPS, EXPERTS_PER_GROUP)
    idx = jnp.broadcast_to(g_idx[:, :, None], (N, 1, EXPERTS_PER_GROUP))
    e_prob = jax.nn.softmax(jnp.take_along_axis(e_logits, idx, axis=1)[:, 0], axis=-1)
    e_val, e_local = lax.top_k(e_prob, EXPERT_TOP_K)
    gates = g_val * e_val / jnp.sum(e_val, axis=-1, keepdims=True)
    expert_ids = g_idx * EXPERTS_PER_GROUP + e_local
    A = N * EXPERT_TOP_K
    eid = expert_ids.reshape(A).astype(jnp.int32)
    tok = jnp.repeat(jnp.arange(N, dtype=jnp.int32), EXPERT_TOP_K)
    gate = gates.reshape(A)
    order = jnp.argsort(eid)
    eid_s, tok_s, gate_s = eid[order], tok[order], gate[order]
    counts = jax.ops.segment_sum(jnp.ones((A,), jnp.int32), eid, num_segments=N_EXPERTS)
    start = jnp.cumsum(counts) - counts
    padded = (counts + EXPERT_BLOCK - 1) // EXPERT_BLOCK * EXPERT_BLOCK
    pad_end = jnp.cumsum(padded)
    pad_start = pad_end - padded
    dest = pad_start[eid_s] + (jnp.arange(A, dtype=jnp.int32) - start[eid_s])
    n_blocks = -(-A // EXPERT_BLOCK) + N_EXPERTS
    buf = jnp.zeros((n_blocks * EXPERT_BLOCK, D), t.dtype).at[dest].set(t[tok_s])
    blk_start = jnp.arange(n_blocks, dtype=jnp.int32) * EXPERT_BLOCK
    blk_expert = jnp.clip(jnp.searchsorted(pad_end, blk_start, side='right'), 0, N_EXPERTS - 1)

    def expert_block(args):
        xb, e = args
        return (jax.nn.silu(xb @ w_gate[e]) * (xb @ w_up[e])) @ w_down[e]

    out = lax.map(expert_block, (buf.reshape(n_blocks, EXPERT_BLOCK, D), blk_expert))
    y_s = out.reshape(n_blocks * EXPERT_BLOCK, D)[dest] * gate_s[:, None].astype(t.dtype)
    y = jnp.zeros((N, D), t.dtype).at[tok_s].add(y_s)
    return y.reshape(B, S, D)


def setup_inputs(seed: int = 0) -> dict:
    key = jax.random.key(seed)
    ks = jax.random.split(key, 24)
    f32 = jnp.float32
    L, D = DEPTH, D_MODEL

    def nrm(k, shape, scale):
        return jax.random.normal(k, shape, f32) * scale

    def gain(k, shape):
        return 1.0 + 0.02 * jax.random.normal(k, shape, f32)

    x = jax.random.normal(ks[0], (BATCH, SEQ, D), f32)
    offsets = jax.random.randint(ks[1], (BATCH, 1), 0, 1000, dtype=jnp.int32) * CHUNK
    positions = (offsets + jnp.arange(SEQ, dtype=jnp.int32)[None, :]).astype(jnp.int32)
    return {
        "x": x,
        "positions": positions,
        "attn_norm": gain(ks[2], (L, D)),
        "w_in": nrm(ks[3], (L, D, IN_WIDTH), D ** -0.5),
        "b_forget": 2.0 + 0.1 * jax.random.normal(ks[4], (L, FOX_HEADS), f32),
        "q_norm": gain(ks[5], (L, MLA_Q_RANK)),
        "w_uq": nrm(ks[6], (L, MLA_Q_RANK, MLA_HEADS * (MLA_NOPE_DIM + MLA_ROPE_DIM)), MLA_Q_RANK ** -0.5),
        "kv_norm": gain(ks[7], (L, MLA_KV_RANK)),
        "w_ukv": nrm(ks[8], (L, MLA_KV_RANK, MLA_HEADS * (MLA_NOPE_DIM + MLA_V_DIM)), MLA_KV_RANK ** -0.5),
        "mla_out_norm": gain(ks[9], (L, MLA_OUT)),
        "fox_out_norm": gain(ks[10], (L, FOX_OUT)),
        "w_out": nrm(ks[11], (L, MIX_WIDTH, D), MIX_WIDTH ** -0.5),
        "ffn_norm": gain(ks[12], (L, D)),
        "w_router_group": nrm(ks[13], (L, D, N_GROUPS), D ** -0.5),
        "b_router_group": nrm(ks[14], (L, N_GROUPS), 0.01),
        "w_router_expert": nrm(ks[15], (L, D, N_EXPERTS), D ** -0.5),
        "b_router_expert": nrm(ks[16], (L, N_EXPERTS), 0.01),
        "w_gate": nrm(ks[17], (L, N_EXPERTS, D, D_EXPERT), D ** -0.5),
        "w_up": nrm(ks[18], (L, N_EXPERTS, D, D_EXPERT), D ** -0.5),
        "w_down": nrm(ks[19], (L, N_EXPERTS, D_EXPERT, D), D_EXPERT ** -0.5),
        "final_norm": gain(ks[20], (D,)),
    }


def reference(x, positions, attn_norm, w_in, b_forget, q_norm, w_uq, kv_norm, w_ukv,
              mla_out_norm, fox_out_norm, w_out, ffn_norm, w_router_group, b_router_group,
              w_router_expert, b_router_expert, w_gate, w_up, w_down, final_norm):
    split_at = np.cumsum([MLA_Q_RANK, MLA_KV_RANK, MLA_ROPE_DIM, FOX_OUT, FOX_OUT, FOX_OUT]).tolist()
    for l in range(DEPTH):
        h = rmsnorm(x, attn_norm[l])
        proj = h @ w_in[l]
        q_lat, kv_lat, k_rope, fq, fk, fv, f_logit = jnp.split(proj, split_at, axis=-1)
        o_mla = mla_group(q_lat, kv_lat, k_rope, positions, q_norm[l], w_uq[l], kv_norm[l], w_ukv[l])
        o_fox = fox_group(fq, fk, fv, f_logit, b_forget[l])
        mixed = jnp.concatenate([rmsnorm(o_mla, mla_out_norm[l]), rmsnorm(o_fox, fox_out_norm[l])], axis=-1)
        x = x + mixed @ w_out[l]
        h2 = rmsnorm(x, ffn_norm[l])
        x = x + hier_moe(h2, w_router_group[l], b_router_group[l], w_router_expert[l],
                         b_router_expert[l], w_gate[l], w_up[l], w_down[l])
    return rmsnorm(x, final_norm)
```

```python
from contextlib import ExitStack

import numpy as np
import ml_dtypes

import concourse.bass as bass
import concourse.mybir as mybir
from concourse.bass_utils import run_bass_kernel_spmd

F32 = mybir.dt.float32
BF16 = mybir.dt.bfloat16
I32 = mybir.dt.int32
AF = mybir.ActivationFunctionType
ALU = mybir.AluOpType

D = 1024
H = 8
INW = 1960
E = 32
G = 4
EPG = 8
DE = 512
EPS = 1e-6
TWO_PI = 2.0 * np.pi
CW1 = 6.28125
CW2 = float(np.float32(np.round((TWO_PI - CW1) * 2 ** 19) / 2 ** 19))
CW3 = float(np.float32(TWO_PI - CW1 - CW2))
MAGIC = 12582912.0


class StopBuild(Exception):
    pass


class Op:
    __slots__ = ("eng", "fn", "deps", "dma_key", "signal", "sem", "val", "idx")

    def __init__(self, eng, fn, dma_key):
        self.eng = eng
        self.fn = fn
        self.deps = set()
        self.dma_key = dma_key
        self.signal = False
        self.sem = None
        self.val = 0


class Sched:
    def __init__(self, nc):
        self.nc = nc
        self.ops = []
        self.lastw = {}
        self.readers = {}
        self.regs = {}

    def reg(self, eng, val):
        k = (id(eng), val)
        if k not in self.regs:
            self.regs[k] = eng.to_reg(val)
        return self.regs[k]

    def add(self, eng, fn, reads=(), writes=(), dma_key=None):
        op = Op(eng, fn, dma_key)
        op.idx = len(self.ops)
        excl = [t for t in reads if isinstance(t, str) and len(t) == 2 and t[0] == "B" and t[1].isdigit()]
        if excl:
            reads = [t for t in reads if t not in excl]
            writes = list(writes) + excl
        for t in reads:
            w = self.lastw.get(t)
            if w is not None:
                op.deps.add(w)
        for t in writes:
            rs = self.readers.get(t, ())
            if rs:
                for r in rs:
                    op.deps.add(r)
            else:
                w = self.lastw.get(t)
                if w is not None:
                    op.deps.add(w)
        for t in reads:
            self.readers.setdefault(t, []).append(op.idx)
        for t in writes:
            self.lastw[t] = op.idx
            self.readers[t] = []
        op.deps.discard(op.idx)
        self.ops.append(op)
        return op

    def emit(self, name):
        nc = self.nc
        ops = self.ops
        for op in ops:
            nd = set()
            for d in op.deps:
                p = ops[d]
                if p.eng == "pe" and op.eng == "pe" and p.dma_key is None and op.dma_key is None:
                    continue
                nd.add(d)
                p.signal = True
            op.deps = nd
        with ExitStack() as es:
            sems = {}
            for e in ("pe", "act", "dve", "pool"):
                sems[e] = es.enter_context(nc.semaphore(f"{name}_{e}"))
            dkeys = sorted({op.dma_key for op in ops if op.dma_key is not None})
            for k in dkeys:
                sems["dma_" + k] = es.enter_context(nc.semaphore(f"{name}_d_{k}"))
            cnt = {k: 0 for k in sems}
            for op in ops:
                if op.dma_key is not None:
                    k = "dma_" + op.dma_key
                    cnt[k] += 16
                    op.sem, op.val = k, cnt[k]
                    op.signal = True
                elif op.signal:
                    cnt[op.eng] += 1
                    op.sem, op.val = op.eng, cnt[op.eng]
            final = dict(cnt)
            block = es.enter_context(nc.Block(name))

            def body(ename):
                def f(eng):
                    known = {}
                    for op in ops:
                        if op.eng != ename:
                            continue
                        need = {}
                        for d in op.deps:
                            p = ops[d]
                            if need.get(p.sem, 0) < p.val:
                                need[p.sem] = p.val
                        for s, v in need.items():
                            if known.get(s, 0) < v:
                                eng.wait_ge(sems[s], v)
                                known[s] = v
                        ins = op.fn(eng)
                        if op.signal:
                            ins.then_inc(sems[op.sem], 16 if op.dma_key is not None else 1)
                    if ename == "sp":
                        for s, v in final.items():
                            if v > 0 and known.get(s, 0) < v:
                                eng.wait_ge(sems[s], v)
                return f

            block.tensor(body("pe"))
            block.scalar(body("act"))
            block.vector(body("dve"))
            block.gpsimd(body("pool"))
            block.sync(body("sp"))


def build(NB, S, CAP, L=2, stop_after=None):
    nc = bass.Bass("TRN2", target_bir_lowering=False)
    NTS = S // 128
    NT = NB * NTS
    NCH = S // 512
    NBLK = CAP // 128
    NSLOT = E * CAP

    def din(name, shape, dt=F32):
        return nc.dram_tensor(name, list(shape), dt, kind="ExternalInput").ap()

    x_d = din("x", [NT * 128, D])
    pos_d = din("pos", [128, NT], I32)
    attn_g = din("attn_g", [L, 128, 8])
    w_in = din("w_in", [L, D, INW])
    b_forget = din("b_forget", [L, H])
    q_g = din("q_g", [L, 128, 2])
    w_uq = din("w_uq", [L, 256, 768])
    kv_g = din("kv_g", [L, 128, 1])
    w_ukv = din("w_ukv", [L, 128, 1024])
    out_g = din("out_g", [L, 128, 8])
    w_out = din("w_out", [L, D, D])
    ffn_g = din("ffn_g", [L, D])
    w_r = din("w_r", [L, D, G + E])
    b_r = din("b_r", [L, G + E])
    w_gate = din("w_gate", [L, E, D, DE])
    w_up = din("w_up", [L, E, D, DE])
    w_down = din("w_down", [L, E, DE, D])
    fin_g = din("fin_g", [D])
    cF_d = din("cF", [128, 128 * 3 + 16 + 32])
    cB_d = din("cB", [128, 128 * 5], BF16)
    out_d = nc.dram_tensor("out", [NT * 128, D], F32, kind="ExternalOutput").ap()
    xres = nc.dram_tensor("xres", [NT * 128, D], F32, kind="Internal").ap()
    xbuf = nc.dram_tensor("xbuf", [NSLOT, D], BF16, kind="Internal").ap()
    ybuf = nc.dram_tensor("ybuf", [NSLOT, D], F32, kind="Internal").ap()

    with ExitStack() as pes:
        uid = [0]

        def uname(name):
            uid[0] += 1
            return f"s{uid[0]}_{name}"

        def PT(name, shape, dt):
            return pes.enter_context(nc.sbuf_tensor(uname(name), list(shape), dt))

        cF = PT("cF", [128, 128 * 3 + 48], F32)
        cB = PT("cB", [128, 640], BF16)
        ident_f = cF[:, 0:128]
        utri_f = cF[:, 128:256]
        ones_f = cF[:, 256:384]
        invfreq = cF[:, 384:400]
        ebase = cF[:, 400:432]
        ident_b = cB[:, 0:128]
        stri_b = cB[:, 128:256]
        ones_b = cB[:, 256:384]
        mask_fox = cB[:, 384:512]
        mask_mla = cB[:, 512:640]
        cos_t = PT("cos_t", [128, NT, 16], F32)
        sin_t = PT("sin_t", [128, NT, 16], F32)
        dest_i = PT("dest_i", [128, NT, 2], I32)
        gates = PT("gates", [128, NT, 2], F32)
        banks = [pes.enter_context(nc.psum_tensor(f"bank{i}", [128, 512], F32)) for i in range(8)]

        def bkf(i):
            return banks[i][:]

        def bkb(i, a):
            return banks[i][:].bitcast(BF16).rearrange("p (a b) -> p a b", a=a)

        with ExitStack() as es:
            def T(name, shape, dt):
                return es.enter_context(nc.sbuf_tensor(uname(name), list(shape), dt))
            Sx = Sched(nc)
            posi = T("posi", [128, NT], I32)
            posf = T("posf", [128, NT], F32)
            ang = T("ang", [128, NT, 16], F32)
            kk = T("kk", [128, NT, 16], F32)
            rr = T("rr", [128, NT, 16], F32)
            zt = T("zt", [128, D], BF16)
            Sx.add("sp", lambda e: e.dma_start(out=cF[:], in_=cF_d), writes=["cF"], dma_key="cF")
            Sx.add("sp", lambda e: e.dma_start(out=cB[:], in_=cB_d), writes=["cB"], dma_key="cB")
            Sx.add("sp", lambda e: e.dma_start(out=posi[:], in_=pos_d), writes=["posi"], dma_key="pos")
            Sx.add("dve", lambda e: e.tensor_copy(out=posf[:], in_=posi[:]), reads=["posi"], writes=["posf"])
            for t in range(NT):
                Sx.add("dve", lambda e, t=t: e.tensor_scalar(out=ang[:, t, :], in0=invfreq, scalar1=posf[:, t:t + 1],
                                                             scalar2=None, op0=ALU.mult),
                       reads=["cF", "posf"], writes=["ang"])
            angf = ang[:].rearrange("p a b -> p (a b)")
            kkf = kk[:].rearrange("p a b -> p (a b)")
            rrf = rr[:].rearrange("p a b -> p (a b)")
            cosf = cos_t[:].rearrange("p a b -> p (a b)")
            sinf = sin_t[:].rearrange("p a b -> p (a b)")
            Sx.add("dve", lambda e: e.tensor_scalar(out=kkf, in0=angf, scalar1=float(1.0 / TWO_PI), scalar2=MAGIC,
                                                    op0=ALU.mult, op1=ALU.add), reads=["ang"], writes=["kk"])
            Sx.add("dve", lambda e: e.tensor_scalar(out=kkf, in0=kkf, scalar1=-MAGIC, scalar2=None, op0=ALU.add),
                   reads=["kk"], writes=["kk"])
            Sx.add("dve", lambda e: e.scalar_tensor_tensor(out=rrf, in0=kkf, scalar=-CW1, in1=angf, op0=ALU.mult, op1=ALU.add),
                   reads=["kk", "ang"], writes=["rr"])
            Sx.add("dve", lambda e: e.scalar_tensor_tensor(out=rrf, in0=kkf, scalar=-CW2, in1=rrf, op0=ALU.mult, op1=ALU.add),
                   reads=["kk", "rr"], writes=["rr"])
            Sx.add("dve", lambda e: e.scalar_tensor_tensor(out=rrf, in0=kkf, scalar=-CW3, in1=rrf, op0=ALU.mult, op1=ALU.add),
                   reads=["kk", "rr"], writes=["rr"])
            Sx.add("dve", lambda e: e.tensor_scalar(out=rrf, in0=rrf, scalar1=float(np.pi), scalar2=float(-np.pi),
                                                    op0=ALU.min, op1=ALU.max), reads=["rr"], writes=["rr"])
            Sx.add("act", lambda e: e.activation(out=sinf, in_=rrf, func=AF.Sin), reads=["rr"], writes=["sin"])
            Sx.add("act", lambda e: e.activation(out=kkf, in_=rrf, func=AF.Sin, scale=0.5), reads=["rr", "kk"], writes=["kk"])
            Sx.add("dve", lambda e: e.tensor_tensor(out=kkf, in0=kkf, in1=kkf, op=ALU.mult), reads=["kk"], writes=["kk"])
            Sx.add("dve", lambda e: e.tensor_scalar(out=cosf, in0=kkf, scalar1=-2.0, scalar2=1.0, op0=ALU.mult, op1=ALU.add),
                   reads=["kk"], writes=["cos"])
            Sx.add("pool", lambda e: e.memset(zt[:], 0.0), writes=["zt"])
            for r0 in range(0, NSLOT, 128):
                Sx.add("sp", lambda e, r0=r0: e.dma_start(out=xbuf[r0:r0 + 128, :], in_=zt[:]), reads=["zt"], dma_key="xz")
            Sx.emit("init")
        if stop_after == ("init", 0):
            return nc

        def combine_tile(Sx, gi, xt, y1, y2, xtok, y1tok, y2tok):
            Sx.add("sp", lambda e: e.dma_start(out=xt, in_=xres[gi * 128:(gi + 1) * 128, :]), writes=[xtok], dma_key=xtok)
            Sx.add("pool", lambda e: e.indirect_dma_start(out=y1, out_offset=None, in_=ybuf,
                                                          in_offset=bass.IndirectOffsetOnAxis(ap=dest_i[:, gi, 0:1], axis=0),
                                                          bounds_check=Sx.reg(e, NSLOT - 1), oob_is_err=False),
                   reads=[("dest", gi)], writes=[y1tok], dma_key=y1tok + "_g")
            Sx.add("pool", lambda e: e.indirect_dma_start(out=y2, out_offset=None, in_=ybuf,
                                                          in_offset=bass.IndirectOffsetOnAxis(ap=dest_i[:, gi, 1:2], axis=0),
                                                          bounds_check=Sx.reg(e, NSLOT - 1), oob_is_err=False),
                   reads=[("dest", gi)], writes=[y2tok], dma_key=y2tok + "_g")
            Sx.add("dve", lambda e: e.scalar_tensor_tensor(out=xt, in0=y1, scalar=gates[:, gi, 0:1], in1=xt, op0=ALU.mult, op1=ALU.add),
                   reads=[xtok, y1tok, ("gate", gi)], writes=[xtok])
            Sx.add("dve", lambda e: e.scalar_tensor_tensor(out=xt, in0=y2, scalar=gates[:, gi, 1:2], in1=xt, op0=ALU.mult, op1=ALU.add),
                   reads=[xtok, y2tok, ("gate", gi)], writes=[xtok])

        def rstd_ops(Sx, src, width, ssum, rstd, junk, rtoks, wtok):
            Sx.add("dve", lambda e: e.scalar_tensor_tensor(out=junk, in0=src, scalar=1.0, in1=src, op0=ALU.mult, op1=ALU.mult, accum_out=ssum),
                   reads=rtoks, writes=["junk", wtok + "_ss"])
            Sx.add("dve", lambda e: e.tensor_scalar(out=ssum, in0=ssum, scalar1=1.0 / width, scalar2=EPS, op0=ALU.mult, op1=ALU.add),
                   reads=[wtok + "_ss"], writes=[wtok + "_ss"])
            Sx.add("act", lambda e: e.activation(out=rstd, in_=ssum, func=AF.Ln), reads=[wtok + "_ss"], writes=[wtok + "_ln"])
            Sx.add("act", lambda e: e.activation(out=rstd, in_=rstd, func=AF.Exp, scale=-0.5), reads=[wtok + "_ln"], writes=[wtok])

        for l in range(L):
            with ExitStack() as es:
                def T(name, shape, dt):
                    return es.enter_context(nc.sbuf_tensor(uname(name), list(shape), dt))
                Sx = Sched(nc)
                win_b = T("win_b", [128, 8, INW], BF16)
                wuq_b = T("wuq_b", [128, 2, 768], BF16)
                wukv_b = T("wukv_b", [128, 1024], BF16)
                wout_b = T("wout_b", [128, 8, D], BF16)
                wr_f = T("wr_f", [128, 8, G + E], F32)
                br_bc = T("br_bc", [128, G + E], F32)
                gffn_bc = T("gffn_bc", [128, D], F32)
                bfg_bc = T("bfg_bc", [128, H], F32)
                gcol = T("gcol", [128, 8 + 2 + 1 + 8], F32)
                KT = T("KT", [128, H, S], BF16)
                Vm = T("Vm", [128, NTS, H, 65], BF16)
                fkT = T("fkT", [128, 4, S], BF16)
                Vf = T("Vf", [128, NTS, H, 65], BF16)
                QT = T("QT", [128, H, 512], BF16)
                fqT = T("fqT", [128, 4, 512], BF16)
                mixed = T("mixed", [128, 4, D], BF16)
                xtb = T("xtb", [128, D], F32)
                Lc = T("Lc", [128, NTS + 1, H], F32)
                ccarry = T("ccarry", [128, E], F32)
                junk = T("junk", [128, D], BF16)
                hb = T("hb", [128, D], BF16)
                h2b = hb
                hT = T("hT", [128, 8, 128], BF16)
                sm = T("sm", [128, 64], F32)
                tm1 = T("tm1", [128, 424], F32)
                qkn = T("qkn", [128, 384], BF16)
                qkT = T("qkT", [128, 3, 128], BF16)
                q_tm = T("q_tm", [128, H, 96], BF16)
                k_tm = T("k_tm", [128, H, 96], BF16)
                rt = T("rt", [128, 4, 4, 16], F32)
                kpe = T("kpe", [128, 32], BF16)
                lsp = T("lsp", [128, 4 * H], F32)
                Pb = [T(f"Pb{i}", [128, 512], BF16) for i in range(4)]
                dbt = T("dbt", [128, 2 * NTS + 2, H], F32)
                Osb = [T(f"Osb{i}", [65, 512], F32) for i in range(2)]
                rden = T("rden", [128, 4], F32)
                mT = hT
                x1 = T("x1", [128, D], F32)
                h2 = T("h2", [128, D], F32)
                h2T = T("h2T", [128, 8, 128], F32)
                lg = T("lg", [128, G + E], F32)
                rs = T("rs", [128, 160], F32)
                asum_b = T("asum_b", [128, E], BF16)

                stopped = False
                def chk(tag):
                    if stop_after == (tag, l):
                        raise StopBuild()
                try:
                    Sx.add("sp", lambda e: e.dma_start(out=gcol[:, 0:8], in_=attn_g[l]), writes=["gcol0"], dma_key="gcol0")
                    Sx.add("sp", lambda e: e.dma_start(out=gcol[:, 8:10], in_=q_g[l]), writes=["gcol1"], dma_key="gcol1")
                    Sx.add("sp", lambda e: e.dma_start(out=gcol[:, 10:11], in_=kv_g[l]), writes=["gcol2"], dma_key="gcol2")
                    Sx.add("sp", lambda e: e.dma_start(out=gcol[:, 11:19], in_=out_g[l]), writes=["gcol3"], dma_key="gcol3")
                    Sx.add("sp", lambda e: e.dma_start(out=wr_f[:], in_=w_r[l].rearrange("(c p) n -> p c n", p=128)), writes=["wr"], dma_key="wr")
                    Sx.add("sp", lambda e: e.dma_start(out=br_bc[:], in_=b_r[l].partition_broadcast(128)), writes=["br"], dma_key="br")
                    Sx.add("sp", lambda e: e.dma_start(out=gffn_bc[:], in_=ffn_g[l].partition_broadcast(128)), writes=["gffn"], dma_key="gffn")
                    Sx.add("sp", lambda e: e.dma_start(out=bfg_bc[:], in_=b_forget[l].partition_broadcast(128)), writes=["bfg"], dma_key="bfg")
                    def load_cast(src, width, dst, gc, gtok):
                        Sx.add("sp", lambda e: e.dma_start(out=x1[:, 0:width], in_=src), writes=["x1"], dma_key="x1")
                        Sx.add("dve", lambda e: e.tensor_scalar(out=dst, in0=x1[:, 0:width], scalar1=gcol[:, gc:gc + 1], scalar2=None, op0=ALU.mult),
                               reads=["x1", gtok], writes=["wts"])
                    for c in range(8):
                        for hf in range(2):
                            load_cast(w_in[l, c * 128:(c + 1) * 128, hf * 980:(hf + 1) * 980], 980, win_b[:, c, hf * 980:(hf + 1) * 980], c, "gcol0")
                    for c in range(2):
                        load_cast(w_uq[l, c * 128:(c + 1) * 128, :], 768, wuq_b[:, c, :], 8 + c, "gcol1")
                    load_cast(w_ukv[l], 1024, wukv_b[:], 10, "gcol2")
                    for c in range(8):
                        load_cast(w_out[l, c * 128:(c + 1) * 128, :], D, wout_b[:, c, :], 11 + c, "gcol3")
                    chk("w")
                    Sx.add("pool", lambda e: e.memset(ccarry[:], 0.0), writes=["ccarry"])
                    Sx.add("pool", lambda e: e.memset(Vm[:, :, :, 64:65], 1.0), writes=["Vm1"])

                    for sq in range(NB):
                        Sx.add("pool", lambda e: e.memset(Lc[:, 0, :], 0.0), reads=["Lc"], writes=["Lc"])
                        for ch in range(NCH):
                            for tt in range(4):
                                ti = ch * 4 + tt
                                gi = sq * NTS + ti
                                xt = xtb[:]
                                xk = "xtb"
                                if l == 0:
                                    Sx.add("sp", lambda e, xt=xt, gi=gi: e.dma_start(out=xt, in_=x_d[gi * 128:(gi + 1) * 128, :]),
                                           writes=[xk + "xt"], dma_key=xk + "xt")
                                else:
                                    combine_tile(Sx, gi, xt, x1[:], h2[:], xk + "xt", "x1", "h2")
                                    Sx.add("sp", lambda e, xt=xt, gi=gi: e.dma_start(out=xres[gi * 128:(gi + 1) * 128, :], in_=xt),
                                           reads=[xk + "xt"], writes=[("xresS", tt)], dma_key=f"xresA{tt}")
                                rstd_ops(Sx, xt, D, sm[:, 0:1], sm[:, 1:2], junk[:], [xk + "xt"], "rs1")
                                Sx.add("dve", lambda e, xt=xt: e.tensor_scalar(out=hb[:], in0=xt, scalar1=sm[:, 1:2], scalar2=None, op0=ALU.mult),
                                       reads=[xk + "xt", "rs1"], writes=["hb"])
                                pT = bkb(0, 8)
                                for c in range(8):
                                    Sx.add("pe", lambda e, c=c: e.transpose(out=pT[:, c, :], in_=hb[:, c * 128:(c + 1) * 128], identity=ident_b),
                                           reads=["hb", "cB"], writes=["B0"])
                                Sx.add("act", lambda e: e.copy(out=hT[:], in_=pT), reads=["B0"], writes=["hT"])
                                for c in range(8):
                                    Sx.add("pe", lambda e, c=c: e.matmul(bkf(1)[:, 0:416], lhsT=hT[:, c, :], rhs=win_b[:, c, 0:416], start=(c == 0), stop=(c == 7)),
                                           reads=["hT", "wts"], writes=["B1"])
                                for c in range(8):
                                    Sx.add("pe", lambda e, c=c: e.matmul(bkf(1)[:, 416:424], lhsT=hT[:, c, :], rhs=win_b[:, c, 1952:1960], start=(c == 0), stop=(c == 7)),
                                           reads=["hT", "wts"], writes=["B1"])
                                for c in range(8):
                                    Sx.add("pe", lambda e, c=c: e.matmul(bkf(2)[:, 0:512], lhsT=hT[:, c, :], rhs=win_b[:, c, 1440:1952], start=(c == 0), stop=(c == 7)),
                                           reads=["hT", "wts"], writes=["B2"])
                                for p in range(4):
                                    for c in range(8):
                                        Sx.add("pe", lambda e, c=c, p=p: e.matmul(bkf(3)[:, p * 128:(p + 1) * 128], lhsT=win_b[:, c, 416 + p * 128:416 + (p + 1) * 128],
                                                                                  rhs=hT[:, c, :], start=(c == 0), stop=(c == 7)),
                                               reads=["hT", "wts"], writes=["B3"])
                                for p in range(4):
                                    for c in range(8):
                                        Sx.add("pe", lambda e, c=c, p=p: e.matmul(bkf(4)[:, p * 128:(p + 1) * 128], lhsT=win_b[:, c, 928 + p * 128:928 + (p + 1) * 128],
                                                                                  rhs=hT[:, c, :], start=(c == 0), stop=(c == 7)),
                                               reads=["hT", "wts"], writes=["B4"])
                                Sx.add("act", lambda e, tt=tt: e.copy(out=fqT[:, :, tt * 128:(tt + 1) * 128], in_=bkf(3).rearrange("p (a b) -> p a b", a=4)),
                                       reads=["B3"], writes=["fqT"])
                                Sx.add("act", lambda e, ti=ti: e.copy(out=fkT[:, :, ti * 128:(ti + 1) * 128], in_=bkf(4).rearrange("p (a b) -> p a b", a=4)),
                                       reads=["B4"], writes=["fkT"])
                                Sx.add("act", lambda e: e.copy(out=tm1[:], in_=bkf(1)[:, 0:424]), reads=["B1"], writes=["tm1"])
                                rstd_ops(Sx, tm1[:, 0:256], 256, sm[:, 2:3], sm[:, 3:4], junk[:, 0:256], ["tm1"], "rsq")
                                rstd_ops(Sx, tm1[:, 256:384], 128, sm[:, 4:5], sm[:, 5:6], junk[:, 256:384], ["tm1"], "rskv")
                                Sx.add("dve", lambda e: e.tensor_scalar(out=qkn[:, 0:256], in0=tm1[:, 0:256], scalar1=sm[:, 3:4], scalar2=None, op0=ALU.mult),
                                       reads=["tm1", "rsq"], writes=["qkn"])
                                Sx.add("dve", lambda e: e.tensor_scalar(out=qkn[:, 256:384], in0=tm1[:, 256:384], scalar1=sm[:, 5:6], scalar2=None, op0=ALU.mult),
                                       reads=["tm1", "rskv"], writes=["qkn"])
                                pT2 = bkb(0, 8)
                                for c in range(3):
                                    Sx.add("pe", lambda e, c=c: e.transpose(out=pT2[:, c, :], in_=qkn[:, c * 128:(c + 1) * 128], identity=ident_b),
                                           reads=["qkn", "cB"], writes=["B0"])
                                Sx.add("act", lambda e: e.copy(out=qkT[:], in_=pT2[:, 0:3, :]), reads=["B0"], writes=["qkT"])
                                for hh in range(2):
                                    for c in range(2):
                                        Sx.add("pe", lambda e, c=c, hh=hh: e.matmul(bkf(5 + hh)[:, 0:384], lhsT=qkT[:, c, :], rhs=wuq_b[:, c, hh * 384:(hh + 1) * 384],
                                                                                    start=(c == 0), stop=(c == 1)),
                                               reads=["qkT", "wts"], writes=[f"B{5 + hh}"])
                                cosb = cos_t[:, gi, :].unsqueeze(1).broadcast_to([128, 4, 16])
                                sinb = sin_t[:, gi, :].unsqueeze(1).broadcast_to([128, 4, 16])
                                for hh in range(2):
                                    qv = bkf(5 + hh)[:, 0:384].rearrange("p (a b) -> p a b", a=4)
                                    bt = f"B{5 + hh}"
                                    Sx.add("act", lambda e, qv=qv, hh=hh: e.copy(out=q_tm[:, hh * 4:(hh + 1) * 4, 0:64], in_=qv[:, :, 0:64]),
                                           reads=[bt], writes=["q_tm"])
                                    Sx.add("dve", lambda e, qv=qv, cosb=cosb: e.tensor_tensor(out=rt[:, 0], in0=qv[:, :, 64:80], in1=cosb, op=ALU.mult), reads=[bt, "cos"], writes=["rt0"])
                                    Sx.add("dve", lambda e, qv=qv, sinb=sinb: e.tensor_tensor(out=rt[:, 1], in0=qv[:, :, 80:96], in1=sinb, op=ALU.mult), reads=[bt, "sin"], writes=["rt1"])
                                    Sx.add("dve", lambda e, qv=qv, sinb=sinb: e.tensor_tensor(out=rt[:, 2], in0=qv[:, :, 64:80], in1=sinb, op=ALU.mult), reads=[bt, "sin"], writes=["rt2"])
                                    Sx.add("dve", lambda e, qv=qv, cosb=cosb: e.tensor_tensor(out=rt[:, 3], in0=qv[:, :, 80:96], in1=cosb, op=ALU.mult), reads=[bt, "cos"], writes=["rt3"])
                                    Sx.add("dve", lambda e, hh=hh: e.tensor_tensor(out=q_tm[:, hh * 4:(hh + 1) * 4, 64:80], in0=rt[:, 0], in1=rt[:, 1], op=ALU.subtract),
                                           reads=["rt0", "rt1"], writes=["q_tm"])
                                    Sx.add("dve", lambda e, hh=hh: e.tensor_tensor(out=q_tm[:, hh * 4:(hh + 1) * 4, 80:96], in0=rt[:, 2], in1=rt[:, 3], op=ALU.add),
                                           reads=["rt2", "rt3"], writes=["q_tm"])
                                c1 = cos_t[:, gi, :]
                                s1 = sin_t[:, gi, :]
                                Sx.add("dve", lambda e, c1=c1: e.tensor_tensor(out=rt[:, 0, 0], in0=tm1[:, 384:400], in1=c1, op=ALU.mult), reads=["tm1", "cos", "q_tm"], writes=["rt0"])
                                Sx.add("dve", lambda e, s1=s1: e.tensor_tensor(out=rt[:, 1, 0], in0=tm1[:, 400:416], in1=s1, op=ALU.mult), reads=["tm1", "sin", "q_tm"], writes=["rt1"])
                                Sx.add("dve", lambda e, s1=s1: e.tensor_tensor(out=rt[:, 2, 0], in0=tm1[:, 384:400], in1=s1, op=ALU.mult), reads=["tm1", "sin", "q_tm"], writes=["rt2"])
                                Sx.add("dve", lambda e, c1=c1: e.tensor_tensor(out=rt[:, 3, 0], in0=tm1[:, 400:416], in1=c1, op=ALU.mult), reads=["tm1", "cos", "q_tm"], writes=["rt3"])
                                Sx.add("dve", lambda e: e.tensor_tensor(out=kpe[:, 0:16], in0=rt[:, 0, 0], in1=rt[:, 1, 0], op=ALU.subtract), reads=["rt0", "rt1"], writes=["kpe"])
                                Sx.add("dve", lambda e: e.tensor_tensor(out=kpe[:, 16:32], in0=rt[:, 2, 0], in1=rt[:, 3, 0], op=ALU.add), reads=["rt2", "rt3"], writes=["kpe"])
                                Sx.add("dve", lambda e: e.tensor_copy(out=k_tm[:, :, 64:96], in_=kpe[:].unsqueeze(1).broadcast_to([128, H, 32])), reads=["kpe"], writes=["k_tm"])
                                pTq = bkb(7, 8)
                                for h in range(H):
                                    Sx.add("pe", lambda e, h=h: e.transpose(out=pTq[0:96, h, :], in_=q_tm[:, h, :], identity=ident_b),
                                           reads=["q_tm", "cB"], writes=["B7"])
                                Sx.add("act", lambda e, tt=tt: e.copy(out=QT[0:96, :, tt * 128:(tt + 1) * 128], in_=pTq[0:96, :, :]), reads=["B7"], writes=["QT"])
                                for hh in range(2):
                                    Sx.add("pe", lambda e, hh=hh: e.matmul(bkf(5 + hh)[:, 0:512], lhsT=qkT[:, 2, :], rhs=wukv_b[:, hh * 512:(hh + 1) * 512], start=True, stop=True),
                                           reads=["qkT", "wts"], writes=[f"B{5 + hh}"])
                                for hh in range(2):
                                    kvv = bkf(5 + hh).rearrange("p (a b) -> p a b", a=4)
                                    bt = f"B{5 + hh}"
                                    Sx.add("act", lambda e, kvv=kvv, hh=hh: e.copy(out=k_tm[:, hh * 4:(hh + 1) * 4, 0:64], in_=kvv[:, :, 0:64]), reads=[bt], writes=["k_tm"])
                                    Sx.add("dve", lambda e, kvv=kvv, hh=hh, ti=ti: e.tensor_copy(out=Vm[:, ti, hh * 4:(hh + 1) * 4, 0:64], in_=kvv[:, :, 64:128]),
                                           reads=[bt], writes=["Vm"])
                                pTk = bkb(7, 8)
                                for h in range(H):
                                    Sx.add("pe", lambda e, h=h: e.transpose(out=pTk[0:96, h, :], in_=k_tm[:, h, :], identity=ident_b),
                                           reads=["k_tm", "cB"], writes=["B7"])
                                Sx.add("act", lambda e, ti=ti: e.copy(out=KT[0:96, :, ti * 128:(ti + 1) * 128], in_=pTk[0:96, :, :]), reads=["B7"], writes=["KT"])
                                Sx.add("dve", lambda e: e.tensor_tensor(out=lsp[:, 0:8], in0=tm1[:, 416:424], in1=bfg_bc[:], op=ALU.add), reads=["tm1", "bfg"], writes=["lsp0"])
                                Sx.add("act", lambda e: e.activation(out=lsp[:, 8:16], in_=lsp[:, 0:8], func=AF.Exp, scale=-1.0), reads=["lsp0"], writes=["lsp1"])
                                Sx.add("act", lambda e: e.activation(out=lsp[:, 16:24], in_=lsp[:, 8:16], func=AF.Ln, bias=1.0, scale=1.0), reads=["lsp1"], writes=["lsp2"])
                                Sx.add("pe", lambda e: e.matmul(bkf(7)[:, 0:8], lhsT=utri_f, rhs=lsp[:, 16:24], start=True, stop=True), reads=["lsp2", "cF"], writes=["B7"])
                                Sx.add("pe", lambda e: e.matmul(bkf(7)[:, 8:16], lhsT=ones_f, rhs=lsp[:, 16:24], start=True, stop=True), reads=["lsp2", "cF"], writes=["B7"])
                                Sx.add("dve", lambda e, ti=ti: e.tensor_tensor(out=Lc[:, ti + 1, :], in0=bkf(7)[:, 8:16], in1=Lc[:, ti, :], op=ALU.add), reads=["B7", "Lc"], writes=["Lc"])
                                Sx.add("dve", lambda e, ti=ti: e.tensor_tensor(out=lsp[:, 24:32], in0=bkf(7)[:, 0:8], in1=Lc[:, ti, :], op=ALU.add), reads=["B7", "Lc"], writes=["lsp3"])
                                Sx.add("dve", lambda e, ti=ti: e.tensor_tensor(out=lsp[:, 24:32], in0=lsp[:, 24:32], in1=Lc[:, ti + 1, :], op=ALU.subtract), reads=["lsp3", "Lc"], writes=["lsp3"])
                                Sx.add("act", lambda e: e.activation(out=lsp[:, 0:8], in_=lsp[:, 24:32], func=AF.Exp), reads=["lsp3", "lsp0", "lsp1"], writes=["lsp0"])
                                Sx.add("dve", lambda e, ti=ti: e.tensor_tensor(out=Vf[:, ti, :, 0:64], in0=bkf(2).rearrange("p (a b) -> p a b", a=H),
                                                                               in1=lsp[:, 0:8].unsqueeze(2).broadcast_to([128, H, 64]), op=ALU.mult),
                                       reads=["B2", "lsp0"], writes=["Vf"])
                                Sx.add("dve", lambda e, ti=ti: e.tensor_copy(out=Vf[:, ti, :, 64:65], in_=lsp[:, 0:8].unsqueeze(2)), reads=["lsp0"], writes=["Vf"])

                            chk("A")
                            q0t = ch * 4
                            rot = [0, 0, 0]

                            def attend(kind, h, qa, qn_):
                                ob = 4 + (rot[1] % 2)
                                rot[1] += 1
                                obt = f"B{ob}"
                                jlast = (qa + qn_) // 128 - 1
                                iend = jlast
                                for j in range(jlast + 1):
                                    qlo = max(qa, j * 128)
                                    n = qa + qn_ - qlo
                                    cq = qlo - q0t * 128
                                    sb = rot[0] % 4
                                    rot[0] += 1
                                    sbt = f"B{sb}"
                                    if kind == "mla":
                                        lhsT = KT[0:96, h, j * 128:(j + 1) * 128]
                                        rhs = QT[0:96, h, cq:cq + n]
                                        rtok = ["KT", "QT"]
                                    else:
                                        pb = (h % 2) * 64
                                        lhsT = fkT[pb:pb + 64, h // 2, j * 128:(j + 1) * 128]
                                        rhs = fqT[pb:pb + 64, h // 2, cq:cq + n]
                                        rtok = ["fkT", "fqT"]
                                    Sx.add("pe", lambda e, sb=sb, n=n, lhsT=lhsT, rhs=rhs: e.matmul(bkf(sb)[:, 0:n], lhsT=lhsT, rhs=rhs, start=True, stop=True),
                                           reads=rtok, writes=[sbt])
                                    pbuf = Pb[sb]
                                    if kind == "mla":
                                        Sx.add("act", lambda e, sb=sb, n=n, pbuf=pbuf: e.activation(out=pbuf[:, 0:n], in_=bkf(sb)[:, 0:n], func=AF.Exp, scale=96 ** -0.5),
                                               reads=[sbt], writes=[f"P{sb}"])
                                    else:
                                        dv, dtok = dbias[(iend, j)]
                                        bap = dv[:, h:h + 1]
                                        Sx.add("act", lambda e, sb=sb, n=n, pbuf=pbuf, bap=bap: e.activation(
                                            out=pbuf[:, 0:n], in_=bkf(sb)[:, 0:n], func=AF.Exp, scale=0.125, bias=bap),
                                            reads=[sbt, dtok], writes=[f"P{sb}"])
                                    if j * 128 >= qa:
                                        mk = mask_mla if kind == "mla" else mask_fox
                                        Sx.add("pool", lambda e, pbuf=pbuf, mk=mk: e.tensor_tensor(out=pbuf[:, 0:128], in0=pbuf[:, 0:128], in1=mk, op=ALU.mult),
                                               reads=[f"P{sb}", "cB"], writes=[f"P{sb}"])
                                    vv = (Vm if kind == "mla" else Vf)[:, j, h, :]
                                    co = qlo - qa
                                    Sx.add("pe", lambda e, ob=ob, co=co, n=n, vv=vv, pbuf=pbuf, j=j: e.matmul(bkf(ob)[0:65, co:co + n], lhsT=vv, rhs=pbuf[:, 0:n],
                                                                                                             start=(j == 0), stop=(j == jlast)),
                                           reads=[f"P{sb}", "Vm" if kind == "mla" else "Vf", "Vm1"], writes=[obt])
                                osb = Osb[ob - 4]
                                ost = f"Osb{ob - 4}"
                                Sx.add("act", lambda e, ob=ob, osb=osb: e.copy(out=osb[:, 0:qn_], in_=bkf(ob)[0:65, 0:qn_]), reads=[obt], writes=[ost])
                                nq = qn_ // 128
                                ptr = bkf(6)[:, 0:nq * 65].rearrange("p (a b) -> p a b", a=nq)
                                for a in range(nq):
                                    Sx.add("pe", lambda e, a=a, osb=osb, ptr=ptr: e.transpose(out=ptr[:, a, :], in_=osb[:, a * 128:(a + 1) * 128], identity=ident_f[0:65, 0:65]),
                                           reads=[ost, "cF"], writes=["B6"])
                                Sx.add("dve", lambda e, ptr=ptr, nq=nq: e.reciprocal(out=rden[:, 0:nq], in_=ptr[:, :, 64]), reads=["B6"], writes=["rden"])
                                col = (0 if kind == "mla" else 512) + h * 64
                                for a in range(nq):
                                    tloc = (qa // 128 - q0t) + a
                                    Sx.add("dve", lambda e, a=a, ptr=ptr, tloc=tloc, col=col: e.tensor_scalar(out=mixed[:, tloc, col:col + 64], in0=ptr[:, a, 0:64],
                                                                                                           scalar1=rden[:, a:a + 1], scalar2=None, op0=ALU.mult),
                                           reads=["B6", "rden"], writes=["mixed"])

                            dbias = {}
                            kdb = 0
                            for sc in range(2):
                                iend = q0t + 2 * sc + 1
                                for j in range(iend + 1):
                                    dv = dbt[:, kdb, :]
                                    kdb += 1
                                    dbias[(iend, j)] = (dv, ("dbias", kdb))
                                    Sx.add("dve", lambda e, dv=dv, j=j, iend=iend: e.tensor_tensor(out=dv, in0=Lc[:, j + 1, :], in1=Lc[:, iend + 1, :], op=ALU.subtract),
                                           reads=["Lc"], writes=[("dbias", kdb)])
                            for h in range(H):
                                attend("mla", h, q0t * 128, 512)
                                for sc in range(2):
                                    attend("fox", h, q0t * 128 + sc * 256, 256)

                            chk("att")
                            for tt in range(4):
                                ti = ch * 4 + tt
                                gi = sq * NTS + ti
                                mx = mixed[:, tt, :]
                                if l == 0:
                                    Sx.add("sp", lambda e, gi=gi: e.dma_start(out=x1[:], in_=x_d[gi * 128:(gi + 1) * 128, :]), writes=["x1"], dma_key="x1")
                                else:
                                    Sx.add("sp", lambda e, gi=gi: e.dma_start(out=x1[:], in_=xres[gi * 128:(gi + 1) * 128, :]), reads=[("xresS", tt)], writes=["x1"], dma_key="x1")
                                rstd_ops(Sx, mx[:, 0:512], 512, sm[:, 8:9], sm[:, 9:10], junk[:, 0:512], ["mixed"], "rsm")
                                rstd_ops(Sx, mx[:, 512:1024], 512, sm[:, 10:11], sm[:, 11:12], junk[:, 512:1024], ["mixed"], "rsf")
                                pTm = bkb(0, 8)
                                for c in range(8):
                                    Sx.add("pe", lambda e, c=c, mx=mx: e.transpose(out=pTm[:, c, :], in_=mx[:, c * 128:(c + 1) * 128], identity=ident_b),
                                           reads=["mixed", "cB"], writes=["B0"])
                                Sx.add("act", lambda e: e.copy(out=mT[:], in_=pTm), reads=["B0"], writes=["hT"])
                                for grp in range(2):
                                    for nh in range(2):
                                        bk = 1 + grp * 2 + nh
                                        for c in range(4):
                                            Sx.add("pe", lambda e, bk=bk, c=c, grp=grp, nh=nh: e.matmul(bkf(bk)[:, 0:512], lhsT=mT[:, grp * 4 + c, :],
                                                                                                         rhs=wout_b[:, grp * 4 + c, nh * 512:(nh + 1) * 512],
                                                                                                         start=(c == 0), stop=(c == 3)),
                                                   reads=["hT", "wts"], writes=[f"B{bk}"])
                                for nh in range(2):
                                    Sx.add("dve", lambda e, nh=nh: e.scalar_tensor_tensor(out=x1[:, nh * 512:(nh + 1) * 512], in0=bkf(1 + nh), scalar=sm[:, 9:10],
                                                                                          in1=x1[:, nh * 512:(nh + 1) * 512], op0=ALU.mult, op1=ALU.add),
                                           reads=[f"B{1 + nh}", "rsm", "x1"], writes=["x1"])
                                    Sx.add("dve", lambda e, nh=nh: e.scalar_tensor_tensor(out=x1[:, nh * 512:(nh + 1) * 512], in0=bkf(3 + nh), scalar=sm[:, 11:12],
                                                                                          in1=x1[:, nh * 512:(nh + 1) * 512], op0=ALU.mult, op1=ALU.add),
                                           reads=[f"B{3 + nh}", "rsf", "x1"], writes=["x1"])
                                Sx.add("sp", lambda e, gi=gi: e.dma_start(out=xres[gi * 128:(gi + 1) * 128, :], in_=x1[:]), reads=["x1"], writes=[("xres", gi)], dma_key="xres")
                                if tt == 1:
                                    chk("B")
                                rstd_ops(Sx, x1[:], D, sm[:, 12:13], sm[:, 13:14], junk[:], ["x1"], "rs2")
                                Sx.add("dve", lambda e: e.scalar_tensor_tensor(out=h2[:], in0=x1[:], scalar=sm[:, 13:14], in1=gffn_bc[:], op0=ALU.mult, op1=ALU.mult),
                                       reads=["x1", "rs2", "gffn"], writes=["h2"])
                                Sx.add("act", lambda e: e.copy(out=h2b[:], in_=h2[:]), reads=["h2"], writes=["hb"])
                                for c in range(8):
                                    bk = 5 + c // 4
                                    Sx.add("pe", lambda e, c=c, bk=bk: e.transpose(out=bkf(bk)[:, (c % 4) * 128:(c % 4 + 1) * 128], in_=h2[:, c * 128:(c + 1) * 128], identity=ident_f),
                                           reads=["h2", "cF"], writes=[f"B{bk}"])
                                Sx.add("act", lambda e: e.copy(out=h2T[:, 0:4, :], in_=bkf(5).rearrange("p (a b) -> p a b", a=4)), reads=["B5"], writes=["h2Ta"])
                                Sx.add("dve", lambda e: e.tensor_copy(out=h2T[:, 4:8, :], in_=bkf(6).rearrange("p (a b) -> p a b", a=4)), reads=["B6"], writes=["h2Tb"])
                                for c in range(8):
                                    Sx.add("pe", lambda e, c=c: e.matmul(bkf(7)[:, 0:G + E], lhsT=h2T[:, c, :], rhs=wr_f[:, c, :], start=(c == 0), stop=(c == 7)),
                                           reads=["h2Ta", "h2Tb", "wr"], writes=["B7"])
                                Sx.add("dve", lambda e: e.tensor_tensor(out=lg[:], in0=bkf(7)[:, 0:G + E], in1=br_bc[:], op=ALU.add), reads=["B7", "br"], writes=["lg"])
                                Sx.add("dve", lambda e: e.tensor_reduce(out=rs[:, 0:1], in_=lg[:, 0:G], axis=mybir.AxisListType.X, op=ALU.max), reads=["lg"], writes=["r_gmax"])
                                Sx.add("dve", lambda e: e.tensor_scalar(out=rs[:, 1:2], in0=rs[:, 0:1], scalar1=-1.0, scalar2=None, op0=ALU.mult), reads=["r_gmax"], writes=["r_ngmax"])
                                Sx.add("act", lambda e: e.activation(out=rs[:, 146:150], in_=lg[:, 0:G], func=AF.Exp, bias=rs[:, 1:2], scale=1.0, accum_out=rs[:, 2:3]),
                                       reads=["lg", "r_ngmax"], writes=["r_sg", "r_j4"])
                                Sx.add("dve", lambda e: e.reciprocal(out=rs[:, 3:4], in_=rs[:, 2:3]), reads=["r_sg"], writes=["r_gval"])
                                Sx.add("dve", lambda e: e.tensor_scalar(out=rs[:, 4:8], in0=lg[:, 0:G], scalar1=rs[:, 0:1], scalar2=None, op0=ALU.is_equal), reads=["lg", "r_gmax"], writes=["r_ohg"])
                                Sx.add("dve", lambda e: e.tensor_scalar(out=rs[:, 8:16], in0=lg[:, G:G + 8], scalar1=rs[:, 4:5], scalar2=None, op0=ALU.mult), reads=["lg", "r_ohg"], writes=["r_esel"])
                                for g in range(1, G):
                                    Sx.add("dve", lambda e, g=g: e.scalar_tensor_tensor(out=rs[:, 8:16], in0=lg[:, G + 8 * g:G + 8 * g + 8], scalar=rs[:, 4 + g:5 + g], in1=rs[:, 8:16],
                                                                                       op0=ALU.mult, op1=ALU.add), reads=["lg", "r_ohg", "r_esel"], writes=["r_esel"])
                                Sx.add("dve", lambda e: e.max(out=rs[:, 16:24], in_=rs[:, 8:16]), reads=["r_esel"], writes=["r_top"])
                                Sx.add("dve", lambda e: e.tensor_tensor(out=rs[:, 24:25], in0=rs[:, 17:18], in1=rs[:, 16:17], op=ALU.subtract), reads=["r_top"], writes=["r_d"])
                                Sx.add("act", lambda e: e.activation(out=rs[:, 25:26], in_=rs[:, 24:25], func=AF.Exp), reads=["r_d"], writes=["r_e2"])
                                Sx.add("dve", lambda e: e.tensor_scalar(out=rs[:, 26:27], in0=rs[:, 25:26], scalar1=1.0, scalar2=None, op0=ALU.add), reads=["r_e2"], writes=["r_den"])
                                Sx.add("dve", lambda e: e.reciprocal(out=rs[:, 27:28], in_=rs[:, 26:27]), reads=["r_den"], writes=["r_rden"])
                                Sx.add("dve", lambda e, gi=gi: e.tensor_tensor(out=gates[:, gi, 0:1], in0=rs[:, 3:4], in1=rs[:, 27:28], op=ALU.mult),
                                       reads=["r_gval", "r_rden", ("gate", gi)], writes=[("gate", gi)])
                                Sx.add("dve", lambda e, gi=gi: e.tensor_tensor(out=gates[:, gi, 1:2], in0=gates[:, gi, 0:1], in1=rs[:, 25:26], op=ALU.mult),
                                       reads=["r_e2", ("gate", gi)], writes=[("gate", gi)])
                                Sx.add("dve", lambda e: e.tensor_scalar(out=rs[:, 32:40], in0=rs[:, 8:16], scalar1=rs[:, 16:17], scalar2=None, op0=ALU.is_equal), reads=["r_esel", "r_top"], writes=["r_oh1"])
                                Sx.add("dve", lambda e: e.tensor_scalar(out=rs[:, 40:48], in0=rs[:, 8:16], scalar1=rs[:, 17:18], scalar2=None, op0=ALU.is_equal), reads=["r_esel", "r_top"], writes=["r_oh2"])
                                for g in range(G):
                                    Sx.add("dve", lambda e, g=g: e.tensor_scalar(out=rs[:, 48 + 8 * g:56 + 8 * g], in0=rs[:, 32:40], scalar1=rs[:, 4 + g:5 + g], scalar2=None, op0=ALU.mult),
                                           reads=["r_oh1", "r_ohg"], writes=["r_A1"])
                                    Sx.add("dve", lambda e, g=g: e.tensor_scalar(out=rs[:, 80 + 8 * g:88 + 8 * g], in0=rs[:, 40:48], scalar1=rs[:, 4 + g:5 + g], scalar2=None, op0=ALU.mult),
                                           reads=["r_oh2", "r_ohg"], writes=["r_A2"])
                                Sx.add("dve", lambda e: e.tensor_tensor(out=asum_b[:], in0=rs[:, 48:80], in1=rs[:, 80:112], op=ALU.add), reads=["r_A1", "r_A2"], writes=["asum"])
                                Sx.add("pe", lambda e: e.matmul(bkf(7)[:, 64:96], lhsT=stri_b, rhs=asum_b[:], start=True, stop=True), reads=["asum", "cB"], writes=["B7"])
                                Sx.add("pe", lambda e: e.matmul(bkf(7)[:, 96:128], lhsT=ones_b, rhs=asum_b[:], start=True, stop=True), reads=["asum", "cB"], writes=["B7"])
                                Sx.add("dve", lambda e: e.tensor_tensor(out=rs[:, 112:144], in0=bkf(7)[:, 64:96], in1=ccarry[:], op=ALU.add), reads=["B7", "ccarry"], writes=["r_slot"])
                                Sx.add("dve", lambda e: e.tensor_tensor(out=ccarry[:], in0=bkf(7)[:, 96:128], in1=ccarry[:], op=ALU.add), reads=["B7", "ccarry", "r_slot"], writes=["ccarry"])
                                Sx.add("dve", lambda e: e.tensor_scalar(out=rs[:, 112:144], in0=rs[:, 112:144], scalar1=float(CAP - 1), scalar2=None, op0=ALU.min), reads=["r_slot"], writes=["r_slot"])
                                Sx.add("dve", lambda e: e.tensor_tensor(out=rs[:, 112:144], in0=rs[:, 112:144], in1=ebase, op=ALU.add), reads=["r_slot", "cF"], writes=["r_slot"])
                                Sx.add("dve", lambda e: e.scalar_tensor_tensor(out=junk[:, 0:32], in0=rs[:, 48:80], scalar=1.0, in1=rs[:, 112:144], op0=ALU.mult, op1=ALU.mult, accum_out=rs[:, 144:145]),
                                       reads=["r_A1", "r_slot"], writes=["r_d1", "junk"])
                                Sx.add("dve", lambda e: e.scalar_tensor_tensor(out=junk[:, 32:64], in0=rs[:, 80:112], scalar=1.0, in1=rs[:, 112:144], op0=ALU.mult, op1=ALU.mult, accum_out=rs[:, 145:146]),
                                       reads=["r_A2", "r_slot"], writes=["r_d2", "junk"])
                                Sx.add("dve", lambda e, gi=gi: e.tensor_copy(out=dest_i[:, gi, :], in_=rs[:, 144:146]), reads=["r_d1", "r_d2", ("dest", gi)], writes=[("dest", gi)])
                                for k in range(2):
                                    Sx.add("pool", lambda e, gi=gi, k=k: e.indirect_dma_start(out=xbuf, out_offset=bass.IndirectOffsetOnAxis(ap=dest_i[:, gi, k:k + 1], axis=0),
                                                                                             in_=h2b[:], in_offset=None, bounds_check=Sx.reg(e, NSLOT - 1), oob_is_err=False),
                                           reads=["hb", ("dest", gi)], writes=[("xbuf", gi, k)], dma_key="xscat")
                except StopBuild:
                    stopped = True
                Sx.emit(f"attn{l}")
            if stop_after == ("attn", l) or stopped:
                return nc

            with ExitStack() as es:
                def T(name, shape, dt):
                    return es.enter_context(nc.sbuf_tensor(uname(name), list(shape), dt))
                Sx = Sched(nc)
                wg_b = [T(f"wg_b{i}", [128, 8, DE], BF16) for i in range(2)]
                wu_b = [T(f"wu_b{i}", [128, 8, DE], BF16) for i in range(2)]
                wd_b = [T(f"wd_b{i}", [128, 4, D], BF16) for i in range(2)]
                xb = [T(f"xb{i}", [128, D], BF16) for i in range(2)]
                xT = T("xT", [128, 8, 128], BF16)
                sg = T("sg", [128, DE], F32)
                hm = T("hm", [128, DE], BF16)
                hmT = T("hmT", [128, 4, 128], BF16)
                ysb = [T(f"ysb{i}", [128, D], F32) for i in range(2)]
                bi = 0
                for ex in range(E):
                    sl = ex % 2
                    for half in range(2):
                        Sx.add("pool", lambda e, ex=ex, sl=sl, half=half: e.dma_start(
                            out=wg_b[sl][:, half * 4:(half + 1) * 4, :], in_=w_gate[l, ex, half * 512:(half + 1) * 512, :].rearrange("(c p) n -> p c n", p=128)),
                            writes=[f"wg{sl}_{half}"], dma_key=f"wg{sl}")
                        Sx.add("pool", lambda e, ex=ex, sl=sl, half=half: e.dma_start(
                            out=wu_b[sl][:, half * 4:(half + 1) * 4, :], in_=w_up[l, ex, half * 512:(half + 1) * 512, :].rearrange("(c p) n -> p c n", p=128)),
                            writes=[f"wu{sl}_{half}"], dma_key=f"wu{sl}")
                        Sx.add("pool", lambda e, ex=ex, sl=sl, half=half: e.dma_start(
                            out=wd_b[sl][:, half * 2:(half + 1) * 2, :], in_=w_down[l, ex, half * 256:(half + 1) * 256, :].rearrange("(c p) n -> p c n", p=128)),
                            writes=[f"wd{sl}_{half}"], dma_key=f"wd{sl}")
                    for jb in range(NBLK):
                        r0 = ex * CAP + jb * 128
                        xs = bi % 2
                        bi += 1
                        Sx.add("sp", lambda e, r0=r0, xs=xs: e.dma_start(out=xb[xs][:], in_=xbuf[r0:r0 + 128, :]), writes=[f"xb{xs}"], dma_key=f"xb{xs}")
                        pT = bkb(0, 8)
                        for c in range(8):
                            Sx.add("pe", lambda e, c=c, xs=xs: e.transpose(out=pT[:, c, :], in_=xb[xs][:, c * 128:(c + 1) * 128], identity=ident_b),
                                   reads=[f"xb{xs}"], writes=["B0"])
                        Sx.add("act", lambda e: e.copy(out=xT[:], in_=pT), reads=["B0"], writes=["xT"])
                        for c in range(8):
                            Sx.add("pe", lambda e, c=c, sl=sl: e.matmul(bkf(1)[:, 0:512], lhsT=xT[:, c, :], rhs=wg_b[sl][:, c, :], start=(c == 0), stop=(c == 7)),
                                   reads=["xT", f"wg{sl}_0", f"wg{sl}_1"], writes=["B1"])
                        for c in range(8):
                            Sx.add("pe", lambda e, c=c, sl=sl: e.matmul(bkf(2)[:, 0:512], lhsT=xT[:, c, :], rhs=wu_b[sl][:, c, :], start=(c == 0), stop=(c == 7)),
                                   reads=["xT", f"wu{sl}_0", f"wu{sl}_1"], writes=["B2"])
                        Sx.add("act", lambda e: e.activation(out=sg[:], in_=bkf(1), func=AF.Silu), reads=["B1"], writes=["sg"])
                        Sx.add("dve", lambda e: e.tensor_tensor(out=hm[:], in0=sg[:], in1=bkf(2), op=ALU.mult), reads=["sg", "B2"], writes=["hm"])
                        pT2 = bkb(3, 8)
                        for c in range(4):
                            Sx.add("pe", lambda e, c=c: e.transpose(out=pT2[:, c, :], in_=hm[:, c * 128:(c + 1) * 128], identity=ident_b), reads=["hm"], writes=["B3"])
                        Sx.add("act", lambda e: e.copy(out=hmT[:], in_=pT2[:, 0:4, :]), reads=["B3"], writes=["hmT"])
                        for nh in range(2):
                            for c in range(4):
                                Sx.add("pe", lambda e, c=c, nh=nh, sl=sl: e.matmul(bkf(4 + nh)[:, 0:512], lhsT=hmT[:, c, :], rhs=wd_b[sl][:, c, nh * 512:(nh + 1) * 512],
                                                                                   start=(c == 0), stop=(c == 3)),
                                       reads=["hmT", f"wd{sl}_0", f"wd{sl}_1"], writes=[f"B{4 + nh}"])
                        Sx.add("act", lambda e, xs=xs: e.copy(out=ysb[xs][:, 0:512], in_=bkf(4)), reads=["B4"], writes=[f"ysa{xs}"])
                        Sx.add("dve", lambda e, xs=xs: e.tensor_copy(out=ysb[xs][:, 512:1024], in_=bkf(5)), reads=["B5"], writes=[f"ysb{xs}"])
                        Sx.add("sp", lambda e, r0=r0, xs=xs: e.dma_start(out=ybuf[r0:r0 + 128, :], in_=ysb[xs][:]), reads=[f"ysa{xs}", f"ysb{xs}"], writes=[f"yo{xs}"], dma_key=f"yo{xs}")
                Sx.emit(f"moe{l}")
            if stop_after == ("moe", l):
                return nc

        with ExitStack() as es:
            def T(name, shape, dt):
                return es.enter_context(nc.sbuf_tensor(uname(name), list(shape), dt))
            Sx = Sched(nc)
            gfin = T("gfin", [128, D], F32)
            xt2 = [T(f"xf{i}", [128, D], F32) for i in range(2)]
            y1f = [T(f"y1f{i}", [128, D], F32) for i in range(2)]
            y2f = [T(f"y2f{i}", [128, D], F32) for i in range(2)]
            junk = T("junkf", [128, D], F32)
            smf = [T(f"smf{i}", [128, 2], F32) for i in range(2)]
            Sx.add("sp", lambda e: e.dma_start(out=gfin[:], in_=fin_g.partition_broadcast(128)), writes=["gfin"], dma_key="gfin")
            for gi in range(NT):
                s_ = gi % 2
                key = f"f{s_}"
                combine_tile(Sx, gi, xt2[s_][:], y1f[s_][:], y2f[s_][:], key + "xt", key + "y1", key + "y2")
                rstd_ops(Sx, xt2[s_][:], D, smf[s_][:, 0:1], smf[s_][:, 1:2], junk[:], [key + "xt"], f"rsf{s_}")
                Sx.add("dve", lambda e, s_=s_: e.scalar_tensor_tensor(out=y1f[s_][:], in0=xt2[s_][:], scalar=smf[s_][:, 1:2], in1=gfin[:], op0=ALU.mult, op1=ALU.mult),
                       reads=[key + "xt", f"rsf{s_}", "gfin", key + "y1"], writes=[key + "y1"])
                Sx.add("sp", lambda e, s_=s_, gi=gi: e.dma_start(out=out_d[gi * 128:(gi + 1) * 128, :], in_=y1f[s_][:]), reads=[key + "y1"], writes=[key + "o"], dma_key=key + "o")
            Sx.emit("final")
    return nc


def host_consts(CAP):
    k = np.arange(128)[:, None]
    m = np.arange(128)[None, :]
    half = 16
    invf = (np.float32(10000.0) ** (-np.arange(half, dtype=np.float32) / np.float32(half))).astype(np.float32)
    cF = np.zeros((128, 128 * 3 + 48), np.float32)
    cF[:, 0:128] = np.eye(128)
    cF[:, 128:256] = (k <= m)
    cF[:, 256:384] = 1.0
    cF[:, 384:400] = invf[None, :]
    cF[:, 400:432] = (np.arange(E) * CAP)[None, :]
    cB = np.zeros((128, 640), np.float32)
    cB[:, 0:128] = np.eye(128)
    cB[:, 128:256] = (k < m)
    cB[:, 256:384] = 1.0
    cB[:, 384:512] = (k <= m)
    cB[:, 512:640] = (k // 64 <= m // 64)
    return cF, cB.astype(ml_dtypes.bfloat16)


def col_layout(v):
    Lh, n = v.shape
    return np.ascontiguousarray(v.reshape(Lh, n // 128, 128).transpose(0, 2, 1))


def make_in_maps(inp, n_cores, NB, S, CAP):
    f = lambda a: np.ascontiguousarray(np.asarray(a, dtype=np.float32))
    cF, cB = host_consts(CAP)
    shared = {
        "attn_g": col_layout(f(inp["attn_norm"])),
        "w_in": f(inp["w_in"]),
        "b_forget": f(inp["b_forget"]),
        "q_g": col_layout(f(inp["q_norm"])),
        "w_uq": f(inp["w_uq"]),
        "kv_g": col_layout(f(inp["kv_norm"])),
        "w_ukv": f(inp["w_ukv"]),
        "out_g": col_layout(np.concatenate([f(inp["mla_out_norm"]), f(inp["fox_out_norm"])], axis=1)),
        "w_out": f(inp["w_out"]),
        "ffn_g": f(inp["ffn_norm"]),
        "w_r": np.ascontiguousarray(np.concatenate([f(inp["w_router_group"]), f(inp["w_router_expert"])], axis=2)),
        "b_r": np.ascontiguousarray(np.concatenate([f(inp["b_router_group"]), f(inp["b_router_expert"])], axis=1)),
        "w_gate": f(inp["w_gate"]),
        "w_up": f(inp["w_up"]),
        "w_down": f(inp["w_down"]),
        "fin_g": f(inp["final_norm"]),
        "cF": cF,
        "cB": cB,
    }
    x = f(inp["x"])
    pos = np.asarray(inp["positions"]).astype(np.int32)
    maps = []
    for c in range(n_cores):
        xs = x[c * NB:(c + 1) * NB].reshape(NB * S, D)
        ps = pos[c * NB:(c + 1) * NB].reshape(NB * S // 128, 128).T
        m = dict(shared)
        m["x"] = np.ascontiguousarray(xs)
        m["pos"] = np.ascontiguousarray(ps)
        maps.append(m)
    return maps


CAP_FULL = 640


def kernel(**inputs):
    n_cores = 8
    B, S, _ = inputs["x"].shape
    NB = B // n_cores
    nc = build(NB, S, CAP_FULL)
    maps = make_in_maps(inputs, n_cores, NB, S, CAP_FULL)
    res = run_bass_kernel_spmd(nc, maps, core_ids=list(range(n_cores)))
    out = np.concatenate([r["out"].reshape(NB, S, D) for r in res.results], axis=0)
    return out.astype(np.float32)
```

```python
from contextlib import ExitStack

import numpy as np
import ml_dtypes

import concourse.bass as bass
import concourse.mybir as mybir
from concourse.bass_utils import run_bass_kernel_spmd

F32 = mybir.dt.float32
BF16 = mybir.dt.bfloat16
I32 = mybir.dt.int32
AF = mybir.ActivationFunctionType
ALU = mybir.AluOpType

D = 1024
H = 8
INW = 1960
E = 32
G = 4
EPG = 8
DE = 512
EPS = 1e-6
TWO_PI = 2.0 * np.pi
CW1 = 6.28125
CW2 = float(np.float32(np.round((TWO_PI - CW1) * 2 ** 19) / 2 ** 19))
CW3 = float(np.float32(TWO_PI - CW1 - CW2))
MAGIC = 12582912.0


class StopBuild(Exception):
    pass


class Op:
    __slots__ = ("eng", "fn", "deps", "dma_key", "signal", "sem", "val", "idx")

    def __init__(self, eng, fn, dma_key):
        self.eng = eng
        self.fn = fn
        self.deps = set()
        self.dma_key = dma_key
        self.signal = False
        self.sem = None
        self.val = 0


class Sched:
    def __init__(self, nc):
        self.nc = nc
        self.ops = []
        self.lastw = {}
        self.readers = {}
        self.regs = {}

    def reg(self, eng, val):
        k = (id(eng), val)
        if k not in self.regs:
            self.regs[k] = eng.to_reg(val)
        return self.regs[k]

    def add(self, eng, fn, reads=(), writes=(), dma_key=None):
        op = Op(eng, fn, dma_key)
        op.idx = len(self.ops)
        excl = [t for t in reads if isinstance(t, str) and len(t) == 2 and t[0] == "B" and t[1].isdigit()]
        if excl:
            reads = [t for t in reads if t not in excl]
            writes = list(writes) + excl
        for t in reads:
            w = self.lastw.get(t)
            if w is not None:
                op.deps.add(w)
        for t in writes:
            rs = self.readers.get(t, ())
            if rs:
                for r in rs:
                    op.deps.add(r)
            else:
                w = self.lastw.get(t)
                if w is not None:
                    op.deps.add(w)
        for t in reads:
            self.readers.setdefault(t, []).append(op.idx)
        for t in writes:
            self.lastw[t] = op.idx
            self.readers[t] = []
        op.deps.discard(op.idx)
        self.ops.append(op)
        return op

    def emit(self, name):
        nc = self.nc
        ops = self.ops
        for op in ops:
            nd = set()
            for d in op.deps:
                p = ops[d]
                if p.eng == "pe" and op.eng == "pe" and p.dma_key is None and op.dma_key is None:
                    continue
                nd.add(d)
                p.signal = True
            op.deps = nd
        with ExitStack() as es:
            sems = {}
            for e in ("pe", "act", "dve", "pool"):
                sems[e] = es.enter_context(nc.semaphore(f"{name}_{e}"))
            dkeys = sorted({op.dma_key for op in ops if op.dma_key is not None})
            for k in dkeys:
                sems["dma_" + k] = es.enter_context(nc.semaphore(f"{name}_d_{k}"))
            cnt = {k: 0 for k in sems}
            for op in ops:
                if op.dma_key is not None:
                    k = "dma_" + op.dma_key
                    cnt[k] += 16
                    op.sem, op.val = k, cnt[k]
                    op.signal = True
                elif op.signal:
                    cnt[op.eng] += 1
                    op.sem, op.val = op.eng, cnt[op.eng]
            final = dict(cnt)
            block = es.enter_context(nc.Block(name))

            def body(ename):
                def f(eng):
                    known = {}
                    for op in ops:
                        if op.eng != ename:
                            continue
                        need = {}
                        for d in op.deps:
                            p = ops[d]
                            if need.get(p.sem, 0) < p.val:
                                need[p.sem] = p.val
                        for s, v in need.items():
                            if known.get(s, 0) < v:
                                eng.wait_ge(sems[s], v)
                                known[s] = v
                        ins = op.fn(eng)
                        if op.signal:
                            ins.then_inc(sems[op.sem], 16 if op.dma_key is not None else 1)
                    if ename == "sp":
                        for s, v in final.items():
                            if v > 0 and known.get(s, 0) < v:
                                eng.wait_ge(sems[s], v)
                return f

            block.tensor(body("pe"))
            block.scalar(body("act"))
            block.vector(body("dve"))
            block.gpsimd(body("pool"))
            block.sync(body("sp"))


def build(NB, S, CAP, L=2, stop_after=None):
    nc = bass.Bass("TRN2", target_bir_lowering=False)
    NTS = S // 128
    NT = NB * NTS
    NCH = S // 512
    NBLK = CAP // 128
    NSLOT = E * CAP

    def din(name, shape, dt=F32):
        return nc.dram_tensor(name, list(shape), dt, kind="ExternalInput").ap()

    x_d = din("x", [NT * 128, D])
    pos_d = din("pos", [128, NT], I32)
    attn_g = din("attn_g", [L, 128, 8])
    w_in = din("w_in", [L, D, INW])
    b_forget = din("b_forget", [L, H])
    q_g = din("q_g", [L, 128, 2])
    w_uq = din("w_uq", [L, 256, 768])
    kv_g = din("kv_g", [L, 128, 1])
    w_ukv = din("w_ukv", [L, 128, 1024])
    out_g = din("out_g", [L, 128, 8])
    w_out = din("w_out", [L, D, D])
    ffn_g = din("ffn_g", [L, D])
    w_r = din("w_r", [L, D, G + E])
    b_r = din("b_r", [L, G + E])
    w_gate = din("w_gate", [L, E, D, DE])
    w_up = din("w_up", [L, E, D, DE])
    w_down = din("w_down", [L, E, DE, D])
    fin_g = din("fin_g", [D])
    cF_d = din("cF", [128, 128 * 3 + 16 + 32])
    cB_d = din("cB", [128, 128 * 5], BF16)
    out_d = nc.dram_tensor("out", [NT * 128, D], F32, kind="ExternalOutput").ap()
    xres = nc.dram_tensor("xres", [NT * 128, D], F32, kind="Internal").ap()
    xbuf = nc.dram_tensor("xbuf", [NSLOT, D], BF16, kind="Internal").ap()
    ybuf = nc.dram_tensor("ybuf", [NSLOT, D], F32, kind="Internal").ap()

    with ExitStack() as pes:
        uid = [0]

        def uname(name):
            uid[0] += 1
            return f"s{uid[0]}_{name}"

        def PT(name, shape, dt):
            return pes.enter_context(nc.sbuf_tensor(uname(name), list(shape), dt))

        cF = PT("cF", [128, 128 * 3 + 48], F32)
        cB = PT("cB", [128, 640], BF16)
        ident_f = cF[:, 0:128]
        utri_f = cF[:, 128:256]
        ones_f = cF[:, 256:384]
        invfreq = cF[:, 384:400]
        ebase = cF[:, 400:432]
        ident_b = cB[:, 0:128]
        stri_b = cB[:, 128:256]
        ones_b = cB[:, 256:384]
        mask_fox = cB[:, 384:512]
        mask_mla = cB[:, 512:640]
        cos_t = PT("cos_t", [128, NT, 16], F32)
        sin_t = PT("sin_t", [128, NT, 16], F32)
        dest_i = PT("dest_i", [128, NT, 2], I32)
        gates = PT("gates", [128, NT, 2], F32)
        banks = [pes.enter_context(nc.psum_tensor(f"bank{i}", [128, 512], F32)) for i in range(8)]

        def bkf(i):
            return banks[i][:]

        def bkb(i, a):
            return banks[i][:].bitcast(BF16).rearrange("p (a b) -> p a b", a=a)

        with ExitStack() as es:
            def T(name, shape, dt):
                return es.enter_context(nc.sbuf_tensor(uname(name), list(shape), dt))
            Sx = Sched(nc)
            posi = T("posi", [128, NT], I32)
            posf = T("posf", [128, NT], F32)
            ang = T("ang", [128, NT, 16], F32)
            kk = T("kk", [128, NT, 16], F32)
            rr = T("rr", [128, NT, 16], F32)
            zt = T("zt", [128, D], BF16)
            Sx.add("sp", lambda e: e.dma_start(out=cF[:], in_=cF_d), writes=["cF"], dma_key="cF")
            Sx.add("sp", lambda e: e.dma_start(out=cB[:], in_=cB_d), writes=["cB"], dma_key="cB")
            Sx.add("sp", lambda e: e.dma_start(out=posi[:], in_=pos_d), writes=["posi"], dma_key="pos")
            Sx.add("dve", lambda e: e.tensor_copy(out=posf[:], in_=posi[:]), reads=["posi"], writes=["posf"])
            for t in range(NT):
                Sx.add("dve", lambda e, t=t: e.tensor_scalar(out=ang[:, t, :], in0=invfreq, scalar1=posf[:, t:t + 1],
                                                             scalar2=None, op0=ALU.mult),
                       reads=["cF", "posf"], writes=["ang"])
            angf = ang[:].rearrange("p a b -> p (a b)")
            kkf = kk[:].rearrange("p a b -> p (a b)")
            rrf = rr[:].rearrange("p a b -> p (a b)")
            cosf = cos_t[:].rearrange("p a b -> p (a b)")
            sinf = sin_t[:].rearrange("p a b -> p (a b)")
            Sx.add("dve", lambda e: e.tensor_scalar(out=kkf, in0=angf, scalar1=float(1.0 / TWO_PI), scalar2=MAGIC,
                                                    op0=ALU.mult, op1=ALU.add), reads=["ang"], writes=["kk"])
            Sx.add("dve", lambda e: e.tensor_scalar(out=kkf, in0=kkf, scalar1=-MAGIC, scalar2=None, op0=ALU.add),
                   reads=["kk"], writes=["kk"])
            Sx.add("dve", lambda e: e.scalar_tensor_tensor(out=rrf, in0=kkf, scalar=-CW1, in1=angf, op0=ALU.mult, op1=ALU.add),
                   reads=["kk", "ang"], writes=["rr"])
            Sx.add("dve", lambda e: e.scalar_tensor_tensor(out=rrf, in0=kkf, scalar=-CW2, in1=rrf, op0=ALU.mult, op1=ALU.add),
                   reads=["kk", "rr"], writes=["rr"])
            Sx.add("dve", lambda e: e.scalar_tensor_tensor(out=rrf, in0=kkf, scalar=-CW3, in1=rrf, op0=ALU.mult, op1=ALU.add),
                   reads=["kk", "rr"], writes=["rr"])
            Sx.add("dve", lambda e: e.tensor_scalar(out=rrf, in0=rrf, scalar1=float(np.pi), scalar2=float(-np.pi),
                                                    op0=ALU.min, op1=ALU.max), reads=["rr"], writes=["rr"])
            Sx.add("act", lambda e: e.activation(out=sinf, in_=rrf, func=AF.Sin), reads=["rr"], writes=["sin"])
            Sx.add("act", lambda e: e.activation(out=kkf, in_=rrf, func=AF.Sin, scale=0.5), reads=["rr", "kk"], writes=["kk"])
            Sx.add("dve", lambda e: e.tensor_tensor(out=kkf, in0=kkf, in1=kkf, op=ALU.mult), reads=["kk"], writes=["kk"])
            Sx.add("dve", lambda e: e.tensor_scalar(out=cosf, in0=kkf, scalar1=-2.0, scalar2=1.0, op0=ALU.mult, op1=ALU.add),
                   reads=["kk"], writes=["cos"])
            Sx.add("pool", lambda e: e.memset(zt[:], 0.0), writes=["zt"])
            for r0 in range(0, NSLOT, 128):
                Sx.add("sp", lambda e, r0=r0: e.dma_start(out=xbuf[r0:r0 + 128, :], in_=zt[:]), reads=["zt"], dma_key="xz")
            Sx.emit("init")
        if stop_after == ("init", 0):
            return nc

        def combine_tile(Sx, gi, xt, y1, y2, xtok, y1tok, y2tok):
            Sx.add("sp", lambda e: e.dma_start(out=xt, in_=xres[gi * 128:(gi + 1) * 128, :]), writes=[xtok], dma_key=xtok)
            Sx.add("pool", lambda e: e.indirect_dma_start(out=y1, out_offset=None, in_=ybuf,
                                                          in_offset=bass.IndirectOffsetOnAxis(ap=dest_i[:, gi, 0:1], axis=0),
                                                          bounds_check=Sx.reg(e, NSLOT - 1), oob_is_err=False),
                   reads=[("dest", gi)], writes=[y1tok], dma_key=y1tok + "_g")
            Sx.add("pool", lambda e: e.indirect_dma_start(out=y2, out_offset=None, in_=ybuf,
                                                          in_offset=bass.IndirectOffsetOnAxis(ap=dest_i[:, gi, 1:2], axis=0),
                                                          bounds_check=Sx.reg(e, NSLOT - 1), oob_is_err=False),
                   reads=[("dest", gi)], writes=[y2tok], dma_key=y2tok + "_g")
            Sx.add("dve", lambda e: e.scalar_tensor_tensor(out=xt, in0=y1, scalar=gates[:, gi, 0:1], in1=xt, op0=ALU.mult, op1=ALU.add),
                   reads=[xtok, y1tok, ("gate", gi)], writes=[xtok])
            Sx.add("dve", lambda e: e.scalar_tensor_tensor(out=xt, in0=y2, scalar=gates[:, gi, 1:2], in1=xt, op0=ALU.mult, op1=ALU.add),
                   reads=[xtok, y2tok, ("gate", gi)], writes=[xtok])

        def rstd_ops(Sx, src, width, ssum, rstd, junk, rtoks, wtok):
            Sx.add("dve", lambda e: e.scalar_tensor_tensor(out=junk, in0=src, scalar=1.0, in1=src, op0=ALU.mult, op1=ALU.mult, accum_out=ssum),
                   reads=rtoks, writes=["junk", wtok + "_ss"])
            Sx.add("dve", lambda e: e.tensor_scalar(out=ssum, in0=ssum, scalar1=1.0 / width, scalar2=EPS, op0=ALU.mult, op1=ALU.add),
                   reads=[wtok + "_ss"], writes=[wtok + "_ss"])
            Sx.add("act", lambda e: e.activation(out=rstd, in_=ssum, func=AF.Ln), reads=[wtok + "_ss"], writes=[wtok + "_ln"])
            Sx.add("act", lambda e: e.activation(out=rstd, in_=rstd, func=AF.Exp, scale=-0.5), reads=[wtok + "_ln"], writes=[wtok])

        for l in range(L):
            with ExitStack() as es:
                def T(name, shape, dt):
                    return es.enter_context(nc.sbuf_tensor(uname(name), list(shape), dt))
                Sx = Sched(nc)
                win_b = T("win_b", [128, 8, INW], BF16)
                wuq_b = T("wuq_b", [128, 2, 768], BF16)
                wukv_b = T("wukv_b", [128, 1024], BF16)
                wout_b = T("wout_b", [128, 8, D], BF16)
                wr_f = T("wr_f", [128, 8, G + E], F32)
                br_bc = T("br_bc", [128, G + E], F32)
                gffn_bc = T("gffn_bc", [128, D], F32)
                bfg_bc = T("bfg_bc", [128, H], F32)
                gcol = T("gcol", [128, 8 + 2 + 1 + 8], F32)
                KT = T("KT", [128, H, S], BF16)
                Vm = T("Vm", [128, NTS, H, 65], BF16)
                fkT = T("fkT", [128, 4, S], BF16)
                Vf = T("Vf", [128, NTS, H, 65], BF16)
                QT = T("QT", [128, H, 512], BF16)
                fqT = T("fqT", [128, 4, 512], BF16)
                mixed = T("mixed", [128, 4, D], BF16)
                xtb = T("xtb", [128, D], F32)
                Lc = T("Lc", [128, NTS + 1, H], F32)
                ccarry = T("ccarry", [128, E], F32)
                junk = T("junk", [128, D], BF16)
                hb = T("hb", [128, D], BF16)
                h2b = hb
                hT = T("hT", [128, 8, 128], BF16)
                sm = T("sm", [128, 64], F32)
                tm1 = T("tm1", [128, 424], F32)
                qkn = T("qkn", [128, 384], BF16)
                qkT = T("qkT", [128, 3, 128], BF16)
                q_tm = T("q_tm", [128, H, 96], BF16)
                k_tm = T("k_tm", [128, H, 96], BF16)
                rt = T("rt", [128, 4, 4, 16], F32)
                kpe = T("kpe", [128, 32], BF16)
                lsp = T("lsp", [128, 4 * H], F32)
                Pb = [T(f"Pb{i}", [128, 512], BF16) for i in range(4)]
                dbt = T("dbt", [128, 2 * NTS + 2, H], F32)
                Osb = [T(f"Osb{i}", [65, 512], F32) for i in range(2)]
                rden = T("rden", [128, 4], F32)
                mT = hT
                x1 = T("x1", [128, D], F32)
                h2 = T("h2", [128, D], F32)
                h2T = T("h2T", [128, 8, 128], F32)
                lg = T("lg", [128, G + E], F32)
                rs = T("rs", [128, 160], F32)
                asum_b = T("asum_b", [128, E], BF16)

                stopped = False
                def chk(tag):
                    if stop_after == (tag, l):
                        raise StopBuild()
                try:
                    Sx.add("sp", lambda e: e.dma_start(out=gcol[:, 0:8], in_=attn_g[l]), writes=["gcol0"], dma_key="gcol0")
                    Sx.add("sp", lambda e: e.dma_start(out=gcol[:, 8:10], in_=q_g[l]), writes=["gcol1"], dma_key="gcol1")
                    Sx.add("sp", lambda e: e.dma_start(out=gcol[:, 10:11], in_=kv_g[l]), writes=["gcol2"], dma_key="gcol2")
                    Sx.add("sp", lambda e: e.dma_start(out=gcol[:, 11:19], in_=out_g[l]), writes=["gcol3"], dma_key="gcol3")
                    Sx.add("sp", lambda e: e.dma_start(out=wr_f[:], in_=w_r[l].rearrange("(c p) n -> p c n", p=128)), writes=["wr"], dma_key="wr")
                    Sx.add("sp", lambda e: e.dma_start(out=br_bc[:], in_=b_r[l].partition_broadcast(128)), writes=["br"], dma_key="br")
                    Sx.add("sp", lambda e: e.dma_start(out=gffn_bc[:], in_=ffn_g[l].partition_broadcast(128)), writes=["gffn"], dma_key="gffn")
                    Sx.add("sp", lambda e: e.dma_start(out=bfg_bc[:], in_=b_forget[l].partition_broadcast(128)), writes=["bfg"], dma_key="bfg")
                    def load_cast(src, width, dst, gc, gtok):
                        Sx.add("sp", lambda e: e.dma_start(out=x1[:, 0:width], in_=src), writes=["x1"], dma_key="x1")
                        Sx.add("dve", lambda e: e.tensor_scalar(out=dst, in0=x1[:, 0:width], scalar1=gcol[:, gc:gc + 1], scalar2=None, op0=ALU.mult),
                               reads=["x1", gtok], writes=["wts"])
                    for c in range(8):
                        for hf in range(2):
                            load_cast(w_in[l, c * 128:(c + 1) * 128, hf * 980:(hf + 1) * 980], 980, win_b[:, c, hf * 980:(hf + 1) * 980], c, "gcol0")
                    for c in range(2):
                        load_cast(w_uq[l, c * 128:(c + 1) * 128, :], 768, wuq_b[:, c, :], 8 + c, "gcol1")
                    load_cast(w_ukv[l], 1024, wukv_b[:], 10, "gcol2")
                    for c in range(8):
                        load_cast(w_out[l, c * 128:(c + 1) * 128, :], D, wout_b[:, c, :], 11 + c, "gcol3")
                    chk("w")
                    Sx.add("pool", lambda e: e.memset(ccarry[:], 0.0), writes=["ccarry"])
                    Sx.add("pool", lambda e: e.memset(Vm[:, :, :, 64:65], 1.0), writes=["Vm1"])

                    for sq in range(NB):
                        Sx.add("pool", lambda e: e.memset(Lc[:, 0, :], 0.0), reads=["Lc"], writes=["Lc"])
                        for ch in range(NCH):
                            for tt in range(4):
                                ti = ch * 4 + tt
                                gi = sq * NTS + ti
                                xt = xtb[:]
                                xk = "xtb"
                                if l == 0:
                                    Sx.add("sp", lambda e, xt=xt, gi=gi: e.dma_start(out=xt, in_=x_d[gi * 128:(gi + 1) * 128, :]),
                                           writes=[xk + "xt"], dma_key=xk + "xt")
                                else:
                                    combine_tile(Sx, gi, xt, x1[:], h2[:], xk + "xt", "x1", "h2")
                                    Sx.add("sp", lambda e, xt=xt, gi=gi: e.dma_start(out=xres[gi * 128:(gi + 1) * 128, :], in_=xt),
                                           reads=[xk + "xt"], writes=[("xresS", tt)], dma_key=f"xresA{tt}")
                                rstd_ops(Sx, xt, D, sm[:, 0:1], sm[:, 1:2], junk[:], [xk + "xt"], "rs1")
                                Sx.add("dve", lambda e, xt=xt: e.tensor_scalar(out=hb[:], in0=xt, scalar1=sm[:, 1:2], scalar2=None, op0=ALU.mult),
                                       reads=[xk + "xt", "rs1"], writes=["hb"])
                                pT = bkb(0, 8)
                                for c in range(8):
                                    Sx.add("pe", lambda e, c=c: e.transpose(out=pT[:, c, :], in_=hb[:, c * 128:(c + 1) * 128], identity=ident_b),
                                           reads=["hb", "cB"], writes=["B0"])
                                Sx.add("act", lambda e: e.copy(out=hT[:], in_=pT), reads=["B0"], writes=["hT"])
                                for c in range(8):
                                    Sx.add("pe", lambda e, c=c: e.matmul(bkf(1)[:, 0:416], lhsT=hT[:, c, :], rhs=win_b[:, c, 0:416], start=(c == 0), stop=(c == 7)),
                                           reads=["hT", "wts"], writes=["B1"])
                                for c in range(8):
                                    Sx.add("pe", lambda e, c=c: e.matmul(bkf(1)[:, 416:424], lhsT=hT[:, c, :], rhs=win_b[:, c, 1952:1960], start=(c == 0), stop=(c == 7)),
                                           reads=["hT", "wts"], writes=["B1"])
                                for c in range(8):
                                    Sx.add("pe", lambda e, c=c: e.matmul(bkf(2)[:, 0:512], lhsT=hT[:, c, :], rhs=win_b[:, c, 1440:1952], start=(c == 0), stop=(c == 7)),
                                           reads=["hT", "wts"], writes=["B2"])
                                for p in range(4):
                                    for c in range(8):
                                        Sx.add("pe", lambda e, c=c, p=p: e.matmul(bkf(3)[:, p * 128:(p + 1) * 128], lhsT=win_b[:, c, 416 + p * 128:416 + (p + 1) * 128],
                                                                                  rhs=hT[:, c, :], start=(c == 0), stop=(c == 7)),
                                               reads=["hT", "wts"], writes=["B3"])
                                for p in range(4):
                                    for c in range(8):
                                        Sx.add("pe", lambda e, c=c, p=p: e.matmul(bkf(4)[:, p * 128:(p + 1) * 128], lhsT=win_b[:, c, 928 + p * 128:928 + (p + 1) * 128],
                                                                                  rhs=hT[:, c, :], start=(c == 0), stop=(c == 7)),
                                               reads=["hT", "wts"], writes=["B4"])
                                Sx.add("act", lambda e, tt=tt: e.copy(out=fqT[:, :, tt * 128:(tt + 1) * 128], in_=bkf(3).rearrange("p (a b) -> p a b", a=4)),
                                       reads=["B3"], writes=["fqT"])
                                Sx.add("act", lambda e, ti=ti: e.copy(out=fkT[:, :, ti * 128:(ti + 1) * 128], in_=bkf(4).rearrange("p (a b) -> p a b", a=4)),
                                       reads=["B4"], writes=["fkT"])
                                Sx.add("act", lambda e: e.copy(out=tm1[:], in_=bkf(1)[:, 0:424]), reads=["B1"], writes=["tm1"])
                                rstd_ops(Sx, tm1[:, 0:256], 256, sm[:, 2:3], sm[:, 3:4], junk[:, 0:256], ["tm1"], "rsq")
                                rstd_ops(Sx, tm1[:, 256:384], 128, sm[:, 4:5], sm[:, 5:6], junk[:, 256:384], ["tm1"], "rskv")
                                Sx.add("dve", lambda e: e.tensor_scalar(out=qkn[:, 0:256], in0=tm1[:, 0:256], scalar1=sm[:, 3:4], scalar2=None, op0=ALU.mult),
                                       reads=["tm1", "rsq"], writes=["qkn"])
                                Sx.add("dve", lambda e: e.tensor_scalar(out=qkn[:, 256:384], in0=tm1[:, 256:384], scalar1=sm[:, 5:6], scalar2=None, op0=ALU.mult),
                                       reads=["tm1", "rskv"], writes=["qkn"])
                                pT2 = bkb(0, 8)
                                for c in range(3):
                                    Sx.add("pe", lambda e, c=c: e.transpose(out=pT2[:, c, :], in_=qkn[:, c * 128:(c + 1) * 128], identity=ident_b),
                                           reads=["qkn", "cB"], writes=["B0"])
                                Sx.add("act", lambda e: e.copy(out=qkT[:], in_=pT2[:, 0:3, :]), reads=["B0"], writes=["qkT"])
                                for hh in range(2):
                                    for c in range(2):
                                        Sx.add("pe", lambda e, c=c, hh=hh: e.matmul(bkf(5 + hh)[:, 0:384], lhsT=qkT[:, c, :], rhs=wuq_b[:, c, hh * 384:(hh + 1) * 384],
                                                                                    start=(c == 0), stop=(c == 1)),
                                               reads=["qkT", "wts"], writes=[f"B{5 + hh}"])
                                cosb = cos_t[:, gi, :].unsqueeze(1).broadcast_to([128, 4, 16])
                                sinb = sin_t[:, gi, :].unsqueeze(1).broadcast_to([128, 4, 16])
                                for hh in range(2):
                                    qv = bkf(5 + hh)[:, 0:384].rearrange("p (a b) -> p a b", a=4)
                                    bt = f"B{5 + hh}"
                                    Sx.add("act", lambda e, qv=qv, hh=hh: e.copy(out=q_tm[:, hh * 4:(hh + 1) * 4, 0:64], in_=qv[:, :, 0:64]),
                                           reads=[bt], writes=["q_tm"])
                                    Sx.add("dve", lambda e, qv=qv, cosb=cosb: e.tensor_tensor(out=rt[:, 0], in0=qv[:, :, 64:80], in1=cosb, op=ALU.mult), reads=[bt, "cos"], writes=["rt0"])
                                    Sx.add("dve", lambda e, qv=qv, sinb=sinb: e.tensor_tensor(out=rt[:, 1], in0=qv[:, :, 80:96], in1=sinb, op=ALU.mult), reads=[bt, "sin"], writes=["rt1"])
                                    Sx.add("dve", lambda e, qv=qv, sinb=sinb: e.tensor_tensor(out=rt[:, 2], in0=qv[:, :, 64:80], in1=sinb, op=ALU.mult), reads=[bt, "sin"], writes=["rt2"])
                                    Sx.add("dve", lambda e, qv=qv, cosb=cosb: e.tensor_tensor(out=rt[:, 3], in0=qv[:, :, 80:96], in1=cosb, op=ALU.mult), reads=[bt, "cos"], writes=["rt3"])
                                    Sx.add("dve", lambda e, hh=hh: e.tensor_tensor(out=q_tm[:, hh * 4:(hh + 1) * 4, 64:80], in0=rt[:, 0], in1=rt[:, 1], op=ALU.subtract),
                                           reads=["rt0", "rt1"], writes=["q_tm"])
                                    Sx.add("dve", lambda e, hh=hh: e.tensor_tensor(out=q_tm[:, hh * 4:(hh + 1) * 4, 80:96], in0=rt[:, 2], in1=rt[:, 3], op=ALU.add),
                                           reads=["rt2", "rt3"], writes=["q_tm"])
                                c1 = cos_t[:, gi, :]
                                s1 = sin_t[:, gi, :]
                                Sx.add("dve", lambda e, c1=c1: e.tensor_tensor(out=rt[:, 0, 0], in0=tm1[:, 384:400], in1=c1, op=ALU.mult), reads=["tm1", "cos", "q_tm"], writes=["rt0"])
                                Sx.add("dve", lambda e, s1=s1: e.tensor_tensor(out=rt[:, 1, 0], in0=tm1[:, 400:416], in1=s1, op=ALU.mult), reads=["tm1", "sin", "q_tm"], writes=["rt1"])
                                Sx.add("dve", lambda e, s1=s1: e.tensor_tensor(out=rt[:, 2, 0], in0=tm1[:, 384:400], in1=s1, op=ALU.mult), reads=["tm1", "sin", "q_tm"], writes=["rt2"])
                                Sx.add("dve", lambda e, c1=c1: e.tensor_tensor(out=rt[:, 3, 0], in0=tm1[:, 400:416], in1=c1, op=ALU.mult), reads=["tm1", "cos", "q_tm"], writes=["rt3"])
                                Sx.add("dve", lambda e: e.tensor_tensor(out=kpe[:, 0:16], in0=rt[:, 0, 0], in1=rt[:, 1, 0], op=ALU.subtract), reads=["rt0", "rt1"], writes=["kpe"])
                                Sx.add("dve", lambda e: e.tensor_tensor(out=kpe[:, 16:32], in0=rt[:, 2, 0], in1=rt[:, 3, 0], op=ALU.add), reads=["rt2", "rt3"], writes=["kpe"])
                                Sx.add("dve", lambda e: e.tensor_copy(out=k_tm[:, :, 64:96], in_=kpe[:].unsqueeze(1).broadcast_to([128, H, 32])), reads=["kpe"], writes=["k_tm"])
                                pTq = bkb(7, 8)
                                for h in range(H):
                                    Sx.add("pe", lambda e, h=h: e.transpose(out=pTq[0:96, h, :], in_=q_tm[:, h, :], identity=ident_b),
                                           reads=["q_tm", "cB"], writes=["B7"])
                                Sx.add("act", lambda e, tt=tt: e.copy(out=QT[0:96, :, tt * 128:(tt + 1) * 128], in_=pTq[0:96, :, :]), reads=["B7"], writes=["QT"])
                                for hh in range(2):
                                    Sx.add("pe", lambda e, hh=hh: e.matmul(bkf(5 + hh)[:, 0:512], lhsT=qkT[:, 2, :], rhs=wukv_b[:, hh * 512:(hh + 1) * 512], start=True, stop=True),
                                           reads=["qkT", "wts"], writes=[f"B{5 + hh}"])
                                for hh in range(2):
                                    kvv = bkf(5 + hh).rearrange("p (a b) -> p a b", a=4)
                                    bt = f"B{5 + hh}"
                                    Sx.add("act", lambda e, kvv=kvv, hh=hh: e.copy(out=k_tm[:, hh * 4:(hh + 1) * 4, 0:64], in_=kvv[:, :, 0:64]), reads=[bt], writes=["k_tm"])
                                    Sx.add("dve", lambda e, kvv=kvv, hh=hh, ti=ti: e.tensor_copy(out=Vm[:, ti, hh * 4:(hh + 1) * 4, 0:64], in_=kvv[:, :, 64:128]),
                                           reads=[bt], writes=["Vm"])
                                pTk = bkb(7, 8)
                                for h in range(H):
                                    Sx.add("pe", lambda e, h=h: e.transpose(out=pTk[0:96, h, :], in_=k_tm[:, h, :], identity=ident_b),
                                           reads=["k_tm", "cB"], writes=["B7"])
                                Sx.add("act", lambda e, ti=ti: e.copy(out=KT[0:96, :, ti * 128:(ti + 1) * 128], in_=pTk[0:96, :, :]), reads=["B7"], writes=["KT"])
                                Sx.add("dve", lambda e: e.tensor_tensor(out=lsp[:, 0:8], in0=tm1[:, 416:424], in1=bfg_bc[:], op=ALU.add), reads=["tm1", "bfg"], writes=["lsp0"])
                                Sx.add("act", lambda e: e.activation(out=lsp[:, 8:16], in_=lsp[:, 0:8], func=AF.Exp, scale=-1.0), reads=["lsp0"], writes=["lsp1"])
                                Sx.add("act", lambda e: e.activation(out=lsp[:, 16:24], in_=lsp[:, 8:16], func=AF.Ln, bias=1.0, scale=1.0), reads=["lsp1"], writes=["lsp2"])
                                Sx.add("pe", lambda e: e.matmul(bkf(7)[:, 0:8], lhsT=utri_f, rhs=lsp[:, 16:24], start=True, stop=True), reads=["lsp2", "cF"], writes=["B7"])
                                Sx.add("pe", lambda e: e.matmul(bkf(7)[:, 8:16], lhsT=ones_f, rhs=lsp[:, 16:24], start=True, stop=True), reads=["lsp2", "cF"], writes=["B7"])
                                Sx.add("dve", lambda e, ti=ti: e.tensor_tensor(out=Lc[:, ti + 1, :], in0=bkf(7)[:, 8:16], in1=Lc[:, ti, :], op=ALU.add), reads=["B7", "Lc"], writes=["Lc"])
                                Sx.add("dve", lambda e, ti=ti: e.tensor_tensor(out=lsp[:, 24:32], in0=bkf(7)[:, 0:8], in1=Lc[:, ti, :], op=ALU.add), reads=["B7", "Lc"], writes=["lsp3"])
                                Sx.add("dve", lambda e, ti=ti: e.tensor_tensor(out=lsp[:, 24:32], in0=lsp[:, 24:32], in1=Lc[:, ti + 1, :], op=ALU.subtract), reads=["lsp3", "Lc"], writes=["lsp3"])
                                Sx.add("act", lambda e: e.activation(out=lsp[:, 0:8], in_=lsp[:, 24:32], func=AF.Exp), reads=["lsp3", "lsp0", "lsp1"], writes=["lsp0"])
                                Sx.add("dve", lambda e, ti=ti: e.tensor_tensor(out=Vf[:, ti, :, 0:64], in0=bkf(2).rearrange("p (a b) -> p a b", a=H),
                                                                               in1=lsp[:, 0:8].unsqueeze(2).broadcast_to([128, H, 64]), op=ALU.mult),
                                       reads=["B2", "lsp0"], writes=["Vf"])
                                Sx.add("dve", lambda e, ti=ti: e.tensor_copy(out=Vf[:, ti, :, 64:65], in_=lsp[:, 0:8].unsqueeze(2)), reads=["lsp0"], writes=["Vf"])

                            chk("A")
                            q0t = ch * 4
                            rot = [0, 0, 0]

                            steps = []

                            def attend(kind, h, qa, qn_):
                                ob = 4 + (rot[1] % 2)
                                rot[1] += 1
                                obt = f"B{ob}"
                                jlast = (qa + qn_) // 128 - 1
                                iend = jlast
                                for j in range(jlast + 1):
                                    front = []
                                    qlo = max(qa, j * 128)
                                    n = qa + qn_ - qlo
                                    cq = qlo - q0t * 128
                                    sb = rot[0] % 4
                                    rot[0] += 1
                                    sbt = f"B{sb}"
                                    if kind == "mla":
                                        lhsT = KT[0:96, h, j * 128:(j + 1) * 128]
                                        rhs = QT[0:96, h, cq:cq + n]
                                        rtok = ["KT", "QT"]
                                    else:
                                        pb = (h % 2) * 64
                                        lhsT = fkT[pb:pb + 64, h // 2, j * 128:(j + 1) * 128]
                                        rhs = fqT[pb:pb + 64, h // 2, cq:cq + n]
                                        rtok = ["fkT", "fqT"]
                                    front.append(("pe", lambda e, sb=sb, n=n, lhsT=lhsT, rhs=rhs: e.matmul(bkf(sb)[:, 0:n], lhsT=lhsT, rhs=rhs, start=True, stop=True),
                                                  rtok, [sbt]))
                                    pbuf = Pb[sb]
                                    if kind == "mla":
                                        front.append(("act", lambda e, sb=sb, n=n, pbuf=pbuf: e.activation(out=pbuf[:, 0:n], in_=bkf(sb)[:, 0:n], func=AF.Exp, scale=96 ** -0.5),
                                                      [sbt], [f"P{sb}"]))
                                    else:
                                        dv, dtok = dbias[(iend, j)]
                                        bap = dv[:, h:h + 1]
                                        front.append(("act", lambda e, sb=sb, n=n, pbuf=pbuf, bap=bap: e.activation(
                                            out=pbuf[:, 0:n], in_=bkf(sb)[:, 0:n], func=AF.Exp, scale=0.125, bias=bap),
                                            [sbt, dtok], [f"P{sb}"]))
                                    if j * 128 >= qa:
                                        mk = mask_mla if kind == "mla" else mask_fox
                                        front.append(("pool", lambda e, pbuf=pbuf, mk=mk: e.tensor_tensor(out=pbuf[:, 0:128], in0=pbuf[:, 0:128], in1=mk, op=ALU.mult),
                                                      [f"P{sb}", "cB"], [f"P{sb}"]))
                                    vv = (Vm if kind == "mla" else Vf)[:, j, h, :]
                                    co = qlo - qa
                                    pv = [("pe", lambda e, ob=ob, co=co, n=n, vv=vv, pbuf=pbuf, j=j, jlast=jlast: e.matmul(bkf(ob)[0:65, co:co + n], lhsT=vv, rhs=pbuf[:, 0:n],
                                                                                                                   start=(j == 0), stop=(j == jlast)),
                                           [f"P{sb}", "Vm" if kind == "mla" else "Vf", "Vm1"], [obt])]
                                    steps.append({"front": front, "pv": pv, "epi": None})
                                epi = []
                                osb = Osb[ob - 4]
                                ost = f"Osb{ob - 4}"
                                epi.append(("act", lambda e, ob=ob, osb=osb: e.copy(out=osb[:, 0:qn_], in_=bkf(ob)[0:65, 0:qn_]), [obt], [ost]))
                                nq = qn_ // 128
                                ptr = bkf(6)[:, 0:nq * 65].rearrange("p (a b) -> p a b", a=nq)
                                for a_ in range(nq):
                                    epi.append(("pe", lambda e, a_=a_, osb=osb, ptr=ptr: e.transpose(out=ptr[:, a_, :], in_=osb[:, a_ * 128:(a_ + 1) * 128], identity=ident_f[0:65, 0:65]),
                                                [ost, "cF"], ["B6"]))
                                epi.append(("dve", lambda e, ptr=ptr, nq=nq: e.reciprocal(out=rden[:, 0:nq], in_=ptr[:, :, 64]), ["B6"], ["rden"]))
                                col = (0 if kind == "mla" else 512) + h * 64
                                for a_ in range(nq):
                                    tloc = (qa // 128 - q0t) + a_
                                    epi.append(("dve", lambda e, a_=a_, ptr=ptr, tloc=tloc, col=col: e.tensor_scalar(out=mixed[:, tloc, col:col + 64], in0=ptr[:, a_, 0:64],
                                                                                                                 scalar1=rden[:, a_:a_ + 1], scalar2=None, op0=ALU.mult),
                                                ["B6", "rden"], ["mixed"]))
                                steps[-1]["epi"] = epi

                            def run_steps(LA=2, EPI_DELAY=1):
                                def emit(ops):
                                    for (eng_, fn_, r_, w_) in ops:
                                        Sx.add(eng_, fn_, reads=r_, writes=w_)
                                pend = []
                                ns = len(steps)
                                for k in range(ns + LA + EPI_DELAY + 1):
                                    if k < ns:
                                        emit(steps[k]["front"])
                                    kp = k - LA
                                    if 0 <= kp < ns:
                                        emit(steps[kp]["pv"])
                                        if steps[kp]["epi"] is not None:
                                            pend.append((k + EPI_DELAY, steps[kp]["epi"]))
                                    while pend and pend[0][0] <= k:
                                        emit(pend.pop(0)[1])
                                for _, ops in pend:
                                    emit(ops)

                            dbias = {}
                            kdb = 0
                            for sc in range(2):
                                iend = q0t + 2 * sc + 1
                                for j in range(iend + 1):
                                    dv = dbt[:, kdb, :]
                                    kdb += 1
                                    dbias[(iend, j)] = (dv, ("dbias", kdb))
                                    Sx.add("dve", lambda e, dv=dv, j=j, iend=iend: e.tensor_tensor(out=dv, in0=Lc[:, j + 1, :], in1=Lc[:, iend + 1, :], op=ALU.subtract),
                                           reads=["Lc"], writes=[("dbias", kdb)])
                            for h in range(H):
                                attend("mla", h, q0t * 128, 512)
                                for sc in range(2):
                                    attend("fox", h, q0t * 128 + sc * 256, 256)
                            run_steps()

                            chk("att")
                            for tt in range(4):
                                ti = ch * 4 + tt
                                gi = sq * NTS + ti
                                mx = mixed[:, tt, :]
                                if l == 0:
                                    Sx.add("sp", lambda e, gi=gi: e.dma_start(out=x1[:], in_=x_d[gi * 128:(gi + 1) * 128, :]), writes=["x1"], dma_key="x1")
                                else:
                                    Sx.add("sp", lambda e, gi=gi: e.dma_start(out=x1[:], in_=xres[gi * 128:(gi + 1) * 128, :]), reads=[("xresS", tt)], writes=["x1"], dma_key="x1")
                                rstd_ops(Sx, mx[:, 0:512], 512, sm[:, 8:9], sm[:, 9:10], junk[:, 0:512], ["mixed"], "rsm")
                                rstd_ops(Sx, mx[:, 512:1024], 512, sm[:, 10:11], sm[:, 11:12], junk[:, 512:1024], ["mixed"], "rsf")
                                pTm = bkb(0, 8)
                                for c in range(8):
                                    Sx.add("pe", lambda e, c=c, mx=mx: e.transpose(out=pTm[:, c, :], in_=mx[:, c * 128:(c + 1) * 128], identity=ident_b),
                                           reads=["mixed", "cB"], writes=["B0"])
                                Sx.add("act", lambda e: e.copy(out=mT[:], in_=pTm), reads=["B0"], writes=["hT"])
                                for grp in range(2):
                                    for nh in range(2):
                                        bk = 1 + grp * 2 + nh
                                        for c in range(4):
                                            Sx.add("pe", lambda e, bk=bk, c=c, grp=grp, nh=nh: e.matmul(bkf(bk)[:, 0:512], lhsT=mT[:, grp * 4 + c, :],
                                                                                                         rhs=wout_b[:, grp * 4 + c, nh * 512:(nh + 1) * 512],
                                                                                                         start=(c == 0), stop=(c == 3)),
                                                   reads=["hT", "wts"], writes=[f"B{bk}"])
                                for nh in range(2):
                                    Sx.add("dve", lambda e, nh=nh: e.scalar_tensor_tensor(out=x1[:, nh * 512:(nh + 1) * 512], in0=bkf(1 + nh), scalar=sm[:, 9:10],
                                                                                          in1=x1[:, nh * 512:(nh + 1) * 512], op0=ALU.mult, op1=ALU.add),
                                           reads=[f"B{1 + nh}", "rsm", "x1"], writes=["x1"])
                                    Sx.add("dve", lambda e, nh=nh: e.scalar_tensor_tensor(out=x1[:, nh * 512:(nh + 1) * 512], in0=bkf(3 + nh), scalar=sm[:, 11:12],
                                                                                          in1=x1[:, nh * 512:(nh + 1) * 512], op0=ALU.mult, op1=ALU.add),
                                           reads=[f"B{3 + nh}", "rsf", "x1"], writes=["x1"])
                                Sx.add("sp", lambda e, gi=gi: e.dma_start(out=xres[gi * 128:(gi + 1) * 128, :], in_=x1[:]), reads=["x1"], writes=[("xres", gi)], dma_key="xres")
                                if tt == 1:
                                    chk("B")
                                rstd_ops(Sx, x1[:], D, sm[:, 12:13], sm[:, 13:14], junk[:], ["x1"], "rs2")
                                Sx.add("dve", lambda e: e.scalar_tensor_tensor(out=h2[:], in0=x1[:], scalar=sm[:, 13:14], in1=gffn_bc[:], op0=ALU.mult, op1=ALU.mult),
                                       reads=["x1", "rs2", "gffn"], writes=["h2"])
                                Sx.add("act", lambda e: e.copy(out=h2b[:], in_=h2[:]), reads=["h2"], writes=["hb"])
                                for c in range(8):
                                    bk = 5 + c // 4
                                    Sx.add("pe", lambda e, c=c, bk=bk: e.transpose(out=bkf(bk)[:, (c % 4) * 128:(c % 4 + 1) * 128], in_=h2[:, c * 128:(c + 1) * 128], identity=ident_f),
                                           reads=["h2", "cF"], writes=[f"B{bk}"])
                                Sx.add("act", lambda e: e.copy(out=h2T[:, 0:4, :], in_=bkf(5).rearrange("p (a b) -> p a b", a=4)), reads=["B5"], writes=["h2Ta"])
                                Sx.add("dve", lambda e: e.tensor_copy(out=h2T[:, 4:8, :], in_=bkf(6).rearrange("p (a b) -> p a b", a=4)), reads=["B6"], writes=["h2Tb"])
                                for c in range(8):
                                    Sx.add("pe", lambda e, c=c: e.matmul(bkf(7)[:, 0:G + E], lhsT=h2T[:, c, :], rhs=wr_f[:, c, :], start=(c == 0), stop=(c == 7)),
                                           reads=["h2Ta", "h2Tb", "wr"], writes=["B7"])
                                Sx.add("dve", lambda e: e.tensor_tensor(out=lg[:], in0=bkf(7)[:, 0:G + E], in1=br_bc[:], op=ALU.add), reads=["B7", "br"], writes=["lg"])
                                Sx.add("dve", lambda e: e.tensor_reduce(out=rs[:, 0:1], in_=lg[:, 0:G], axis=mybir.AxisListType.X, op=ALU.max), reads=["lg"], writes=["r_gmax"])
                                Sx.add("dve", lambda e: e.tensor_scalar(out=rs[:, 1:2], in0=rs[:, 0:1], scalar1=-1.0, scalar2=None, op0=ALU.mult), reads=["r_gmax"], writes=["r_ngmax"])
                                Sx.add("act", lambda e: e.activation(out=rs[:, 146:150], in_=lg[:, 0:G], func=AF.Exp, bias=rs[:, 1:2], scale=1.0, accum_out=rs[:, 2:3]),
                                       reads=["lg", "r_ngmax"], writes=["r_sg", "r_j4"])
                                Sx.add("dve", lambda e: e.reciprocal(out=rs[:, 3:4], in_=rs[:, 2:3]), reads=["r_sg"], writes=["r_gval"])
                                Sx.add("dve", lambda e: e.tensor_scalar(out=rs[:, 4:8], in0=lg[:, 0:G], scalar1=rs[:, 0:1], scalar2=None, op0=ALU.is_equal), reads=["lg", "r_gmax"], writes=["r_ohg"])
                                Sx.add("dve", lambda e: e.tensor_scalar(out=rs[:, 8:16], in0=lg[:, G:G + 8], scalar1=rs[:, 4:5], scalar2=None, op0=ALU.mult), reads=["lg", "r_ohg"], writes=["r_esel"])
                                for g in range(1, G):
                                    Sx.add("dve", lambda e, g=g: e.scalar_tensor_tensor(out=rs[:, 8:16], in0=lg[:, G + 8 * g:G + 8 * g + 8], scalar=rs[:, 4 + g:5 + g], in1=rs[:, 8:16],
                                                                                       op0=ALU.mult, op1=ALU.add), reads=["lg", "r_ohg", "r_esel"], writes=["r_esel"])
                                Sx.add("dve", lambda e: e.max(out=rs[:, 16:24], in_=rs[:, 8:16]), reads=["r_esel"], writes=["r_top"])
                                Sx.add("dve", lambda e: e.tensor_tensor(out=rs[:, 24:25], in0=rs[:, 17:18], in1=rs[:, 16:17], op=ALU.subtract), reads=["r_top"], writes=["r_d"])
                                Sx.add("act", lambda e: e.activation(out=rs[:, 25:26], in_=rs[:, 24:25], func=AF.Exp), reads=["r_d"], writes=["r_e2"])
                                Sx.add("dve", lambda e: e.tensor_scalar(out=rs[:, 26:27], in0=rs[:, 25:26], scalar1=1.0, scalar2=None, op0=ALU.add), reads=["r_e2"], writes=["r_den"])
                                Sx.add("dve", lambda e: e.reciprocal(out=rs[:, 27:28], in_=rs[:, 26:27]), reads=["r_den"], writes=["r_rden"])
                                Sx.add("dve", lambda e, gi=gi: e.tensor_tensor(out=gates[:, gi, 0:1], in0=rs[:, 3:4], in1=rs[:, 27:28], op=ALU.mult),
                                       reads=["r_gval", "r_rden", ("gate", gi)], writes=[("gate", gi)])
                                Sx.add("dve", lambda e, gi=gi: e.tensor_tensor(out=gates[:, gi, 1:2], in0=gates[:, gi, 0:1], in1=rs[:, 25:26], op=ALU.mult),
                                       reads=["r_e2", ("gate", gi)], writes=[("gate", gi)])
                                Sx.add("dve", lambda e: e.tensor_scalar(out=rs[:, 32:40], in0=rs[:, 8:16], scalar1=rs[:, 16:17], scalar2=None, op0=ALU.is_equal), reads=["r_esel", "r_top"], writes=["r_oh1"])
                                Sx.add("dve", lambda e: e.tensor_scalar(out=rs[:, 40:48], in0=rs[:, 8:16], scalar1=rs[:, 17:18], scalar2=None, op0=ALU.is_equal), reads=["r_esel", "r_top"], writes=["r_oh2"])
                                for g in range(G):
                                    Sx.add("dve", lambda e, g=g: e.tensor_scalar(out=rs[:, 48 + 8 * g:56 + 8 * g], in0=rs[:, 32:40], scalar1=rs[:, 4 + g:5 + g], scalar2=None, op0=ALU.mult),
                                           reads=["r_oh1", "r_ohg"], writes=["r_A1"])
                                    Sx.add("dve", lambda e, g=g: e.tensor_scalar(out=rs[:, 80 + 8 * g:88 + 8 * g], in0=rs[:, 40:48], scalar1=rs[:, 4 + g:5 + g], scalar2=None, op0=ALU.mult),
                                           reads=["r_oh2", "r_ohg"], writes=["r_A2"])
                                Sx.add("dve", lambda e: e.tensor_tensor(out=asum_b[:], in0=rs[:, 48:80], in1=rs[:, 80:112], op=ALU.add), reads=["r_A1", "r_A2"], writes=["asum"])
                                Sx.add("pe", lambda e: e.matmul(bkf(7)[:, 64:96], lhsT=stri_b, rhs=asum_b[:], start=True, stop=True), reads=["asum", "cB"], writes=["B7"])
                                Sx.add("pe", lambda e: e.matmul(bkf(7)[:, 96:128], lhsT=ones_b, rhs=asum_b[:], start=True, stop=True), reads=["asum", "cB"], writes=["B7"])
                                Sx.add("dve", lambda e: e.tensor_tensor(out=rs[:, 112:144], in0=bkf(7)[:, 64:96], in1=ccarry[:], op=ALU.add), reads=["B7", "ccarry"], writes=["r_slot"])
                                Sx.add("dve", lambda e: e.tensor_tensor(out=ccarry[:], in0=bkf(7)[:, 96:128], in1=ccarry[:], op=ALU.add), reads=["B7", "ccarry", "r_slot"], writes=["ccarry"])
                                Sx.add("dve", lambda e: e.tensor_scalar(out=rs[:, 112:144], in0=rs[:, 112:144], scalar1=float(CAP - 1), scalar2=None, op0=ALU.min), reads=["r_slot"], writes=["r_slot"])
                                Sx.add("dve", lambda e: e.tensor_tensor(out=rs[:, 112:144], in0=rs[:, 112:144], in1=ebase, op=ALU.add), reads=["r_slot", "cF"], writes=["r_slot"])
                                Sx.add("dve", lambda e: e.scalar_tensor_tensor(out=junk[:, 0:32], in0=rs[:, 48:80], scalar=1.0, in1=rs[:, 112:144], op0=ALU.mult, op1=ALU.mult, accum_out=rs[:, 144:145]),
                                       reads=["r_A1", "r_slot"], writes=["r_d1", "junk"])
                                Sx.add("dve", lambda e: e.scalar_tensor_tensor(out=junk[:, 32:64], in0=rs[:, 80:112], scalar=1.0, in1=rs[:, 112:144], op0=ALU.mult, op1=ALU.mult, accum_out=rs[:, 145:146]),
                                       reads=["r_A2", "r_slot"], writes=["r_d2", "junk"])
                                Sx.add("dve", lambda e, gi=gi: e.tensor_copy(out=dest_i[:, gi, :], in_=rs[:, 144:146]), reads=["r_d1", "r_d2", ("dest", gi)], writes=[("dest", gi)])
                                for k in range(2):
                                    Sx.add("pool", lambda e, gi=gi, k=k: e.indirect_dma_start(out=xbuf, out_offset=bass.IndirectOffsetOnAxis(ap=dest_i[:, gi, k:k + 1], axis=0),
                                                                                             in_=h2b[:], in_offset=None, bounds_check=Sx.reg(e, NSLOT - 1), oob_is_err=False),
                                           reads=["hb", ("dest", gi)], writes=[("xbuf", gi, k)], dma_key="xscat")
                except StopBuild:
                    stopped = True
                Sx.emit(f"attn{l}")
            if stop_after == ("attn", l) or stopped:
                return nc

            with ExitStack() as es:
                def T(name, shape, dt):
                    return es.enter_context(nc.sbuf_tensor(uname(name), list(shape), dt))
                Sx = Sched(nc)
                wg_b = [T(f"wg_b{i}", [128, 8, DE], BF16) for i in range(2)]
                wu_b = [T(f"wu_b{i}", [128, 8, DE], BF16) for i in range(2)]
                wd_b = [T(f"wd_b{i}", [128, 4, D], BF16) for i in range(2)]
                xb = [T(f"xb{i}", [128, D], BF16) for i in range(2)]
                xT = T("xT", [128, 8, 128], BF16)
                hmT = T("hmT", [128, 4, 128], BF16)
                ysb = [T(f"ysb{i}", [128, D], F32) for i in range(2)]
                sg = [T(f"sg{i}", [128, DE], F32) for i in range(2)]
                hm = [T(f"hm{i}", [128, DE], BF16) for i in range(2)]
                blocks = []
                for ex in range(E):
                    for jb in range(NBLK):
                        blocks.append((ex, ex % 2, jb, ex * CAP + jb * 128))

                def wload(ex):
                    sl = ex % 2
                    for half in range(2):
                        Sx.add("pool", lambda e, ex=ex, sl=sl, half=half: e.dma_start(
                            out=wg_b[sl][:, half * 4:(half + 1) * 4, :], in_=w_gate[l, ex, half * 512:(half + 1) * 512, :].rearrange("(c p) n -> p c n", p=128)),
                            writes=[f"wg{sl}_{half}"], dma_key=f"wg{sl}")
                        Sx.add("pool", lambda e, ex=ex, sl=sl, half=half: e.dma_start(
                            out=wu_b[sl][:, half * 4:(half + 1) * 4, :], in_=w_up[l, ex, half * 512:(half + 1) * 512, :].rearrange("(c p) n -> p c n", p=128)),
                            writes=[f"wu{sl}_{half}"], dma_key=f"wu{sl}")
                        Sx.add("pool", lambda e, ex=ex, sl=sl, half=half: e.dma_start(
                            out=wd_b[sl][:, half * 2:(half + 1) * 2, :], in_=w_down[l, ex, half * 256:(half + 1) * 256, :].rearrange("(c p) n -> p c n", p=128)),
                            writes=[f"wd{sl}_{half}"], dma_key=f"wd{sl}")

                def s_load(bi_):
                    ex, sl, jb, r0 = blocks[bi_]
                    xs = bi_ % 2
                    Sx.add("sp", lambda e, r0=r0, xs=xs: e.dma_start(out=xb[xs][:], in_=xbuf[r0:r0 + 128, :]), writes=[f"xb{xs}"], dma_key=f"xb{xs}")

                def s_T(bi_):
                    xs = bi_ % 2
                    pT = bkb(0, 8)
                    for c in range(8):
                        Sx.add("pe", lambda e, c=c, xs=xs, pT=pT: e.transpose(out=pT[:, c, :], in_=xb[xs][:, c * 128:(c + 1) * 128], identity=ident_b),
                               reads=[f"xb{xs}"], writes=["B0"])
                    Sx.add("act", lambda e, pT=pT: e.copy(out=xT[:], in_=pT), reads=["B0"], writes=["xT"])

                def s_GU(bi_):
                    ex, sl, jb, r0 = blocks[bi_]
                    k2 = bi_ % 2
                    for c in range(8):
                        Sx.add("pe", lambda e, c=c, sl=sl: e.matmul(bkf(1)[:, 0:512], lhsT=xT[:, c, :], rhs=wg_b[sl][:, c, :], start=(c == 0), stop=(c == 7)),
                               reads=["xT", f"wg{sl}_0", f"wg{sl}_1"], writes=["B1"])
                    for c in range(8):
                        Sx.add("pe", lambda e, c=c, sl=sl: e.matmul(bkf(2)[:, 0:512], lhsT=xT[:, c, :], rhs=wu_b[sl][:, c, :], start=(c == 0), stop=(c == 7)),
                               reads=["xT", f"wu{sl}_0", f"wu{sl}_1"], writes=["B2"])
                    Sx.add("act", lambda e, k2=k2: e.activation(out=sg[k2][:], in_=bkf(1), func=AF.Silu), reads=["B1"], writes=[f"sg{k2}"])
                    Sx.add("dve", lambda e, k2=k2: e.tensor_tensor(out=hm[k2][:], in0=sg[k2][:], in1=bkf(2), op=ALU.mult), reads=[f"sg{k2}", "B2"], writes=[f"hm{k2}"])

                def s_T2(bi_):
                    k2 = bi_ % 2
                    pT2 = bkb(3, 8)
                    for c in range(4):
                        Sx.add("pe", lambda e, c=c, k2=k2, pT2=pT2: e.transpose(out=pT2[:, c, :], in_=hm[k2][:, c * 128:(c + 1) * 128], identity=ident_b), reads=[f"hm{k2}"], writes=["B3"])
                    Sx.add("act", lambda e, pT2=pT2: e.copy(out=hmT[:], in_=pT2[:, 0:4, :]), reads=["B3"], writes=["hmT"])

                def s_D(bi_):
                    ex, sl, jb, r0 = blocks[bi_]
                    xs = bi_ % 2
                    for nh in range(2):
                        for c in range(4):
                            Sx.add("pe", lambda e, c=c, nh=nh, sl=sl: e.matmul(bkf(4 + nh)[:, 0:512], lhsT=hmT[:, c, :], rhs=wd_b[sl][:, c, nh * 512:(nh + 1) * 512],
                                                                               start=(c == 0), stop=(c == 3)),
                                   reads=["hmT", f"wd{sl}_0", f"wd{sl}_1"], writes=[f"B{4 + nh}"])
                    Sx.add("act", lambda e, xs=xs: e.copy(out=ysb[xs][:, 0:512], in_=bkf(4)), reads=["B4"], writes=[f"ysa{xs}"])
                    Sx.add("dve", lambda e, xs=xs: e.tensor_copy(out=ysb[xs][:, 512:1024], in_=bkf(5)), reads=["B5"], writes=[f"ysb{xs}"])
                    Sx.add("sp", lambda e, r0=r0, xs=xs: e.dma_start(out=ybuf[r0:r0 + 128, :], in_=ysb[xs][:]), reads=[f"ysa{xs}", f"ysb{xs}"], writes=[f"yo{xs}"], dma_key=f"yo{xs}")

                NB_ = len(blocks)
                wload(0)
                wload(1)
                s_load(0)
                s_load(1)
                s_T(0)
                s_GU(0)
                for bi_ in range(NB_):
                    nxt = bi_ + 1
                    if nxt < NB_:
                        if nxt + 1 < NB_:
                            s_load(nxt + 1)
                        s_T(nxt)
                    s_T2(bi_)
                    if nxt < NB_:
                        s_GU(nxt)
                    s_D(bi_)
                    ex, sl, jb, r0 = blocks[bi_]
                    if jb == NBLK - 1 and ex + 2 < E:
                        wload(ex + 2)
                Sx.emit(f"moe{l}")
            if stop_after == ("moe", l):
                return nc

        with ExitStack() as es:
            def T(name, shape, dt):
                return es.enter_context(nc.sbuf_tensor(uname(name), list(shape), dt))
            Sx = Sched(nc)
            gfin = T("gfin", [128, D], F32)
            xt2 = [T(f"xf{i}", [128, D], F32) for i in range(2)]
            y1f = [T(f"y1f{i}", [128, D], F32) for i in range(2)]
            y2f = [T(f"y2f{i}", [128, D], F32) for i in range(2)]
            junk = T("junkf", [128, D], F32)
            smf = [T(f"smf{i}", [128, 2], F32) for i in range(2)]
            Sx.add("sp", lambda e: e.dma_start(out=gfin[:], in_=fin_g.partition_broadcast(128)), writes=["gfin"], dma_key="gfin")
            for gi in range(NT):
                s_ = gi % 2
                key = f"f{s_}"
                combine_tile(Sx, gi, xt2[s_][:], y1f[s_][:], y2f[s_][:], key + "xt", key + "y1", key + "y2")
                rstd_ops(Sx, xt2[s_][:], D, smf[s_][:, 0:1], smf[s_][:, 1:2], junk[:], [key + "xt"], f"rsf{s_}")
                Sx.add("dve", lambda e, s_=s_: e.scalar_tensor_tensor(out=y1f[s_][:], in0=xt2[s_][:], scalar=smf[s_][:, 1:2], in1=gfin[:], op0=ALU.mult, op1=ALU.mult),
                       reads=[key + "xt", f"rsf{s_}", "gfin", key + "y1"], writes=[key + "y1"])
                Sx.add("sp", lambda e, s_=s_, gi=gi: e.dma_start(out=out_d[gi * 128:(gi + 1) * 128, :], in_=y1f[s_][:]), reads=[key + "y1"], writes=[key + "o"], dma_key=key + "o")
            Sx.emit("final")
    return nc


def host_consts(CAP):
    k = np.arange(128)[:, None]
    m = np.arange(128)[None, :]
    half = 16
    invf = (np.float32(10000.0) ** (-np.arange(half, dtype=np.float32) / np.float32(half))).astype(np.float32)
    cF = np.zeros((128, 128 * 3 + 48), np.float32)
    cF[:, 0:128] = np.eye(128)
    cF[:, 128:256] = (k <= m)
    cF[:, 256:384] = 1.0
    cF[:, 384:400] = invf[None, :]
    cF[:, 400:432] = (np.arange(E) * CAP)[None, :]
    cB = np.zeros((128, 640), np.float32)
    cB[:, 0:128] = np.eye(128)
    cB[:, 128:256] = (k < m)
    cB[:, 256:384] = 1.0
    cB[:, 384:512] = (k <= m)
    cB[:, 512:640] = (k // 64 <= m // 64)
    return cF, cB.astype(ml_dtypes.bfloat16)


def col_layout(v):
    Lh, n = v.shape
    return np.ascontiguousarray(v.reshape(Lh, n // 128, 128).transpose(0, 2, 1))


def make_in_maps(inp, n_cores, NB, S, CAP):
    f = lambda a: np.ascontiguousarray(np.asarray(a, dtype=np.float32))
    cF, cB = host_consts(CAP)
    shared = {
        "attn_g": col_layout(f(inp["attn_norm"])),
        "w_in": f(inp["w_in"]),
        "b_forget": f(inp["b_forget"]),
        "q_g": col_layout(f(inp["q_norm"])),
        "w_uq": f(inp["w_uq"]),
        "kv_g": col_layout(f(inp["kv_norm"])),
        "w_ukv": f(inp["w_ukv"]),
        "out_g": col_layout(np.concatenate([f(inp["mla_out_norm"]), f(inp["fox_out_norm"])], axis=1)),
        "w_out": f(inp["w_out"]),
        "ffn_g": f(inp["ffn_norm"]),
        "w_r": np.ascontiguousarray(np.concatenate([f(inp["w_router_group"]), f(inp["w_router_expert"])], axis=2)),
        "b_r": np.ascontiguousarray(np.concatenate([f(inp["b_router_group"]), f(inp["b_router_expert"])], axis=1)),
        "w_gate": f(inp["w_gate"]),
        "w_up": f(inp["w_up"]),
        "w_down": f(inp["w_down"]),
        "fin_g": f(inp["final_norm"]),
        "cF": cF,
        "cB": cB,
    }
    x = f(inp["x"])
    pos = np.asarray(inp["positions"]).astype(np.int32)
    maps = []
    for c in range(n_cores):
        xs = x[c * NB:(c + 1) * NB].reshape(NB * S, D)
        ps = pos[c * NB:(c + 1) * NB].reshape(NB * S // 128, 128).T
        m = dict(shared)
        m["x"] = np.ascontiguousarray(xs)
        m["pos"] = np.ascontiguousarray(ps)
        maps.append(m)
    return maps


CAP_FULL = 640


def kernel(**inputs):
    n_cores = 8
    B, S, _ = inputs["x"].shape
    NB = B // n_cores
    nc = build(NB, S, CAP_FULL)
    maps = make_in_maps(inputs, n_cores, NB, S, CAP_FULL)
    res = run_bass_kernel_spmd(nc, maps, core_ids=list(range(n_cores)))
    out = np.concatenate([r["out"].reshape(NB, S, D) for r in res.results], axis=0)
    return out.astype(np.float32)
```

```python
from contextlib import ExitStack

import numpy as np
import ml_dtypes

import concourse.bass as bass
import concourse.mybir as mybir
from concourse.bass_utils import run_bass_kernel_spmd

F32 = mybir.dt.float32
BF16 = mybir.dt.bfloat16
I32 = mybir.dt.int32
AF = mybir.ActivationFunctionType
ALU = mybir.AluOpType

D = 1024
H = 8
INW = 1960
E = 32
G = 4
EPG = 8
DE = 512
EPS = 1e-6
TWO_PI = 2.0 * np.pi
CW1 = 6.28125
CW2 = float(np.float32(np.round((TWO_PI - CW1) * 2 ** 19) / 2 ** 19))
CW3 = float(np.float32(TWO_PI - CW1 - CW2))
MAGIC = 12582912.0


class StopBuild(Exception):
    pass


class Op:
    __slots__ = ("eng", "fn", "deps", "dma_key", "signal", "sem", "val", "idx")

    def __init__(self, eng, fn, dma_key):
        self.eng = eng
        self.fn = fn
        self.deps = set()
        self.dma_key = dma_key
        self.signal = False
        self.sem = None
        self.val = 0


class Sched:
    def __init__(self, nc):
        self.nc = nc
        self.ops = []
        self.lastw = {}
        self.readers = {}
        self.regs = {}

    def reg(self, eng, val):
        k = (id(eng), val)
        if k not in self.regs:
            self.regs[k] = eng.to_reg(val)
        return self.regs[k]

    def add(self, eng, fn, reads=(), writes=(), dma_key=None):
        op = Op(eng, fn, dma_key)
        op.idx = len(self.ops)
        excl = [t for t in reads if isinstance(t, str) and len(t) == 2 and t[0] == "B" and t[1].isdigit()]
        if excl:
            reads = [t for t in reads if t not in excl]
            writes = list(writes) + excl
        for t in reads:
            w = self.lastw.get(t)
            if w is not None:
                op.deps.add(w)
        for t in writes:
            rs = self.readers.get(t, ())
            if rs:
                for r in rs:
                    op.deps.add(r)
            else:
                w = self.lastw.get(t)
                if w is not None:
                    op.deps.add(w)
        for t in reads:
            self.readers.setdefault(t, []).append(op.idx)
        for t in writes:
            self.lastw[t] = op.idx
            self.readers[t] = []
        op.deps.discard(op.idx)
        self.ops.append(op)
        return op

    def emit(self, name):
        nc = self.nc
        ops = self.ops
        for op in ops:
            nd = set()
            for d in op.deps:
                p = ops[d]
                if p.eng == "pe" and op.eng == "pe" and p.dma_key is None and op.dma_key is None:
                    continue
                nd.add(d)
                p.signal = True
            op.deps = nd
        with ExitStack() as es:
            sems = {}
            for e in ("pe", "act", "dve", "pool"):
                sems[e] = es.enter_context(nc.semaphore(f"{name}_{e}"))
            dkeys = sorted({op.dma_key for op in ops if op.dma_key is not None})
            for k in dkeys:
                sems["dma_" + k] = es.enter_context(nc.semaphore(f"{name}_d_{k}"))
            cnt = {k: 0 for k in sems}
            for op in ops:
                if op.dma_key is not None:
                    k = "dma_" + op.dma_key
                    cnt[k] += 16
                    op.sem, op.val = k, cnt[k]
                    op.signal = True
                elif op.signal:
                    cnt[op.eng] += 1
                    op.sem, op.val = op.eng, cnt[op.eng]
            final = dict(cnt)
            block = es.enter_context(nc.Block(name))

            def body(ename):
                def f(eng):
                    known = {}
                    for op in ops:
                        if op.eng != ename:
                            continue
                        need = {}
                        for d in op.deps:
                            p = ops[d]
                            if need.get(p.sem, 0) < p.val:
                                need[p.sem] = p.val
                        for s, v in need.items():
                            if known.get(s, 0) < v:
                                eng.wait_ge(sems[s], v)
                                known[s] = v
                        ins = op.fn(eng)
                        if op.signal:
                            ins.then_inc(sems[op.sem], 16 if op.dma_key is not None else 1)
                    if ename == "sp":
                        for s, v in final.items():
                            if v > 0 and known.get(s, 0) < v:
                                eng.wait_ge(sems[s], v)
                return f

            block.tensor(body("pe"))
            block.scalar(body("act"))
            block.vector(body("dve"))
            block.gpsimd(body("pool"))
            block.sync(body("sp"))


def build(NB, S, CAP, L=2, stop_after=None):
    nc = bass.Bass("TRN2", target_bir_lowering=False)
    NTS = S // 128
    NT = NB * NTS
    NCH = S // 512
    NBLK = CAP // 128
    NSLOT = E * CAP

    def din(name, shape, dt=F32):
        return nc.dram_tensor(name, list(shape), dt, kind="ExternalInput").ap()

    x_d = din("x", [NT * 128, D])
    pos_d = din("pos", [128, NT], I32)
    attn_g = din("attn_g", [L, 128, 8])
    w_in = din("w_in", [L, D, INW])
    b_forget = din("b_forget", [L, H])
    q_g = din("q_g", [L, 128, 2])
    w_uq = din("w_uq", [L, 256, 768])
    kv_g = din("kv_g", [L, 128, 1])
    w_ukv = din("w_ukv", [L, 128, 1024])
    out_g = din("out_g", [L, 128, 8])
    w_out = din("w_out", [L, D, D])
    ffn_g = din("ffn_g", [L, D])
    w_r = din("w_r", [L, D, G + E])
    b_r = din("b_r", [L, G + E])
    w_gate = din("w_gate", [L, E, D, DE])
    w_up = din("w_up", [L, E, D, DE])
    w_down = din("w_down", [L, E, DE, D])
    fin_g = din("fin_g", [D])
    cF_d = din("cF", [128, 128 * 3 + 16 + 32])
    cB_d = din("cB", [128, 128 * 5], BF16)
    out_d = nc.dram_tensor("out", [NT * 128, D], F32, kind="ExternalOutput").ap()
    xres = nc.dram_tensor("xres", [NT * 128, D], F32, kind="Internal").ap()
    xbuf = nc.dram_tensor("xbuf", [NSLOT, D], BF16, kind="Internal").ap()
    ybuf = nc.dram_tensor("ybuf", [NSLOT, D], F32, kind="Internal").ap()

    with ExitStack() as pes:
        uid = [0]

        def uname(name):
            uid[0] += 1
            return f"s{uid[0]}_{name}"

        def PT(name, shape, dt):
            return pes.enter_context(nc.sbuf_tensor(uname(name), list(shape), dt))

        cF = PT("cF", [128, 128 * 3 + 48], F32)
        cB = PT("cB", [128, 640], BF16)
        ident_f = cF[:, 0:128]
        utri_f = cF[:, 128:256]
        ones_f = cF[:, 256:384]
        invfreq = cF[:, 384:400]
        ebase = cF[:, 400:432]
        ident_b = cB[:, 0:128]
        stri_b = cB[:, 128:256]
        ones_b = cB[:, 256:384]
        mask_fox = cB[:, 384:512]
        mask_mla = cB[:, 512:640]
        cos_t = PT("cos_t", [128, NT, 16], F32)
        sin_t = PT("sin_t", [128, NT, 16], F32)
        dest_i = PT("dest_i", [128, NT, 2], I32)
        gates = PT("gates", [128, NT, 2], F32)
        banks = [pes.enter_context(nc.psum_tensor(f"bank{i}", [128, 512], F32)) for i in range(8)]

        def bkf(i):
            return banks[i][:]

        def bkb(i, a):
            return banks[i][:].bitcast(BF16).rearrange("p (a b) -> p a b", a=a)

        with ExitStack() as es:
            def T(name, shape, dt):
                return es.enter_context(nc.sbuf_tensor(uname(name), list(shape), dt))
            Sx = Sched(nc)
            posi = T("posi", [128, NT], I32)
            posf = T("posf", [128, NT], F32)
            ang = T("ang", [128, NT, 16], F32)
            kk = T("kk", [128, NT, 16], F32)
            rr = T("rr", [128, NT, 16], F32)
            zt = T("zt", [128, D], BF16)
            Sx.add("sp", lambda e: e.dma_start(out=cF[:], in_=cF_d), writes=["cF"], dma_key="cF")
            Sx.add("sp", lambda e: e.dma_start(out=cB[:], in_=cB_d), writes=["cB"], dma_key="cB")
            Sx.add("sp", lambda e: e.dma_start(out=posi[:], in_=pos_d), writes=["posi"], dma_key="pos")
            Sx.add("dve", lambda e: e.tensor_copy(out=posf[:], in_=posi[:]), reads=["posi"], writes=["posf"])
            for t in range(NT):
                Sx.add("dve", lambda e, t=t: e.tensor_scalar(out=ang[:, t, :], in0=invfreq, scalar1=posf[:, t:t + 1],
                                                             scalar2=None, op0=ALU.mult),
                       reads=["cF", "posf"], writes=["ang"])
            angf = ang[:].rearrange("p a b -> p (a b)")
            kkf = kk[:].rearrange("p a b -> p (a b)")
            rrf = rr[:].rearrange("p a b -> p (a b)")
            cosf = cos_t[:].rearrange("p a b -> p (a b)")
            sinf = sin_t[:].rearrange("p a b -> p (a b)")
            Sx.add("dve", lambda e: e.tensor_scalar(out=kkf, in0=angf, scalar1=float(1.0 / TWO_PI), scalar2=MAGIC,
                                                    op0=ALU.mult, op1=ALU.add), reads=["ang"], writes=["kk"])
            Sx.add("dve", lambda e: e.tensor_scalar(out=kkf, in0=kkf, scalar1=-MAGIC, scalar2=None, op0=ALU.add),
                   reads=["kk"], writes=["kk"])
            Sx.add("dve", lambda e: e.scalar_tensor_tensor(out=rrf, in0=kkf, scalar=-CW1, in1=angf, op0=ALU.mult, op1=ALU.add),
                   reads=["kk", "ang"], writes=["rr"])
            Sx.add("dve", lambda e: e.scalar_tensor_tensor(out=rrf, in0=kkf, scalar=-CW2, in1=rrf, op0=ALU.mult, op1=ALU.add),
                   reads=["kk", "rr"], writes=["rr"])
            Sx.add("dve", lambda e: e.scalar_tensor_tensor(out=rrf, in0=kkf, scalar=-CW3, in1=rrf, op0=ALU.mult, op1=ALU.add),
                   reads=["kk", "rr"], writes=["rr"])
            Sx.add("dve", lambda e: e.tensor_scalar(out=rrf, in0=rrf, scalar1=float(np.pi), scalar2=float(-np.pi),
                                                    op0=ALU.min, op1=ALU.max), reads=["rr"], writes=["rr"])
            Sx.add("act", lambda e: e.activation(out=sinf, in_=rrf, func=AF.Sin), reads=["rr"], writes=["sin"])
            Sx.add("act", lambda e: e.activation(out=kkf, in_=rrf, func=AF.Sin, scale=0.5), reads=["rr", "kk"], writes=["kk"])
            Sx.add("dve", lambda e: e.tensor_tensor(out=kkf, in0=kkf, in1=kkf, op=ALU.mult), reads=["kk"], writes=["kk"])
            Sx.add("dve", lambda e: e.tensor_scalar(out=cosf, in0=kkf, scalar1=-2.0, scalar2=1.0, op0=ALU.mult, op1=ALU.add),
                   reads=["kk"], writes=["cos"])
            Sx.add("pool", lambda e: e.memset(zt[:], 0.0), writes=["zt"])
            for r0 in range(0, NSLOT, 128):
                Sx.add("sp", lambda e, r0=r0: e.dma_start(out=xbuf[r0:r0 + 128, :], in_=zt[:]), reads=["zt"], dma_key="xz")
            Sx.emit("init")
        if stop_after == ("init", 0):
            return nc

        def combine_tile(Sx, gi, xt, y1, y2, xtok, y1tok, y2tok, xkey=None):
            Sx.add("sp", lambda e: e.dma_start(out=xt, in_=xres[gi * 128:(gi + 1) * 128, :]), writes=[xtok], dma_key=(xkey or xtok))
            Sx.add("pool", lambda e: e.indirect_dma_start(out=y1, out_offset=None, in_=ybuf,
                                                          in_offset=bass.IndirectOffsetOnAxis(ap=dest_i[:, gi, 0:1], axis=0),
                                                          bounds_check=Sx.reg(e, NSLOT - 1), oob_is_err=False),
                   reads=[("dest", gi)], writes=[y1tok], dma_key=y1tok + "_g")
            Sx.add("pool", lambda e: e.indirect_dma_start(out=y2, out_offset=None, in_=ybuf,
                                                          in_offset=bass.IndirectOffsetOnAxis(ap=dest_i[:, gi, 1:2], axis=0),
                                                          bounds_check=Sx.reg(e, NSLOT - 1), oob_is_err=False),
                   reads=[("dest", gi)], writes=[y2tok], dma_key=y2tok + "_g")
            Sx.add("dve", lambda e: e.scalar_tensor_tensor(out=xt, in0=y1, scalar=gates[:, gi, 0:1], in1=xt, op0=ALU.mult, op1=ALU.add),
                   reads=[xtok, y1tok, ("gate", gi)], writes=[xtok])
            Sx.add("dve", lambda e: e.scalar_tensor_tensor(out=xt, in0=y2, scalar=gates[:, gi, 1:2], in1=xt, op0=ALU.mult, op1=ALU.add),
                   reads=[xtok, y2tok, ("gate", gi)], writes=[xtok])

        def rstd_ops(Sx, src, width, ssum, rstd, junk, rtoks, wtok):
            Sx.add("dve", lambda e: e.scalar_tensor_tensor(out=junk, in0=src, scalar=1.0, in1=src, op0=ALU.mult, op1=ALU.mult, accum_out=ssum),
                   reads=rtoks, writes=["junk", wtok + "_ss"])
            Sx.add("dve", lambda e: e.tensor_scalar(out=ssum, in0=ssum, scalar1=1.0 / width, scalar2=EPS, op0=ALU.mult, op1=ALU.add),
                   reads=[wtok + "_ss"], writes=[wtok + "_ss"])
            Sx.add("act", lambda e: e.activation(out=rstd, in_=ssum, func=AF.Ln), reads=[wtok + "_ss"], writes=[wtok + "_ln"])
            Sx.add("act", lambda e: e.activation(out=rstd, in_=rstd, func=AF.Exp, scale=-0.5), reads=[wtok + "_ln"], writes=[wtok])

        for l in range(L):
            with ExitStack() as es:
                def T(name, shape, dt):
                    return es.enter_context(nc.sbuf_tensor(uname(name), list(shape), dt))
                Sx = Sched(nc)
                win_b = T("win_b", [128, 8, INW], BF16)
                wuq_b = T("wuq_b", [128, 2, 768], BF16)
                wukv_b = T("wukv_b", [128, 1024], BF16)
                wout_b = T("wout_b", [128, 8, D], BF16)
                wr_f = T("wr_f", [128, 8, G + E], F32)
                br_bc = T("br_bc", [128, G + E], F32)
                gffn_bc = T("gffn_bc", [128, D], F32)
                bfg_bc = T("bfg_bc", [128, H], F32)
                gcol = T("gcol", [128, 8 + 2 + 1 + 8], F32)
                KT = T("KT", [128, H, S], BF16)
                Vm = T("Vm", [128, NTS, H, 65], BF16)
                fkT = T("fkT", [128, 4, S], BF16)
                Vf = T("Vf", [128, NTS, H, 65], BF16)
                QT = T("QT", [128, H, 512], BF16)
                fqT = T("fqT", [128, 4, 512], BF16)
                mixed = T("mixed", [128, 4, D], BF16)
                xtb_ap = mixed[:, 0:2, :].rearrange("p a b -> p (a b)").bitcast(F32)
                Lc = T("Lc", [128, NTS + 1, H], F32)
                ccarry = T("ccarry", [128, E], F32)
                junk = T("junk", [128, D], BF16)
                hb = T("hb", [128, D], BF16)
                h2b = hb
                hT = T("hT", [128, 8, 128], BF16)
                sm = T("sm", [128, 64], F32)
                tm1 = T("tm1", [128, 424], F32)
                qkn = T("qkn", [128, 384], BF16)
                qkT = T("qkT", [128, 3, 128], BF16)
                q_tm = T("q_tm", [128, H, 96], BF16)
                k_tm = T("k_tm", [128, H, 96], BF16)
                rt = T("rt", [128, 4, 4, 16], F32)
                kpe = T("kpe", [128, 32], BF16)
                lsp = T("lsp", [128, 4 * H], F32)
                Pb = [T(f"Pb{i}", [128, 512], BF16) for i in range(4)]
                dbt = T("dbt", [128, 2 * NTS + 2, H], F32)
                Osb = [T(f"Osb{i}", [65, 512], F32) for i in range(2)]
                rden = T("rden", [128, 4], F32)
                mT = hT
                x1b = [T(f"x1_{i}", [128, D], F32) for i in range(2)]
                h2 = T("h2", [128, D], F32)
                h2T = T("h2T", [128, 8, 128], F32)
                lg = T("lg", [128, G + E], F32)
                rs = T("rs", [128, 160], F32)
                asum_b = T("asum_b", [128, E], BF16)

                stopped = False
                def chk(tag):
                    if stop_after == (tag, l):
                        raise StopBuild()
                try:
                    Sx.add("sp", lambda e: e.dma_start(out=gcol[:, 0:8], in_=attn_g[l]), writes=["gcol0"], dma_key="gcol0")
                    Sx.add("sp", lambda e: e.dma_start(out=gcol[:, 8:10], in_=q_g[l]), writes=["gcol1"], dma_key="gcol1")
                    Sx.add("sp", lambda e: e.dma_start(out=gcol[:, 10:11], in_=kv_g[l]), writes=["gcol2"], dma_key="gcol2")
                    Sx.add("sp", lambda e: e.dma_start(out=gcol[:, 11:19], in_=out_g[l]), writes=["gcol3"], dma_key="gcol3")
                    Sx.add("sp", lambda e: e.dma_start(out=wr_f[:], in_=w_r[l].rearrange("(c p) n -> p c n", p=128)), writes=["wr"], dma_key="wr")
                    Sx.add("sp", lambda e: e.dma_start(out=br_bc[:], in_=b_r[l].partition_broadcast(128)), writes=["br"], dma_key="br")
                    Sx.add("sp", lambda e: e.dma_start(out=gffn_bc[:], in_=ffn_g[l].partition_broadcast(128)), writes=["gffn"], dma_key="gffn")
                    Sx.add("sp", lambda e: e.dma_start(out=bfg_bc[:], in_=b_forget[l].partition_broadcast(128)), writes=["bfg"], dma_key="bfg")
                    def load_cast(src, width, dst, gc, gtok):
                        Sx.add("sp", lambda e: e.dma_start(out=x1b[0][:, 0:width], in_=src), writes=["x1_0"], dma_key="x1_0")
                        Sx.add("dve", lambda e: e.tensor_scalar(out=dst, in0=x1b[0][:, 0:width], scalar1=gcol[:, gc:gc + 1], scalar2=None, op0=ALU.mult),
                               reads=["x1_0", gtok], writes=["wts"])
                    for c in range(8):
                        for hf in range(2):
                            load_cast(w_in[l, c * 128:(c + 1) * 128, hf * 980:(hf + 1) * 980], 980, win_b[:, c, hf * 980:(hf + 1) * 980], c, "gcol0")
                    for c in range(2):
                        load_cast(w_uq[l, c * 128:(c + 1) * 128, :], 768, wuq_b[:, c, :], 8 + c, "gcol1")
                    load_cast(w_ukv[l], 1024, wukv_b[:], 10, "gcol2")
                    for c in range(8):
                        load_cast(w_out[l, c * 128:(c + 1) * 128, :], D, wout_b[:, c, :], 11 + c, "gcol3")
                    chk("w")
                    Sx.add("pool", lambda e: e.memset(ccarry[:], 0.0), writes=["ccarry"])
                    Sx.add("pool", lambda e: e.memset(Vm[:, :, :, 64:65], 1.0), writes=["Vm1"])

                    for sq in range(NB):
                        Sx.add("pool", lambda e: e.memset(Lc[:, 0, :], 0.0), reads=["Lc"], writes=["Lc"])
                        for ch in range(NCH):
                            for tt in range(4):
                                ti = ch * 4 + tt
                                gi = sq * NTS + ti
                                xt = xtb_ap
                                xk = "xtb"
                                if l == 0:
                                    Sx.add("sp", lambda e, xt=xt, gi=gi: e.dma_start(out=xt, in_=x_d[gi * 128:(gi + 1) * 128, :]),
                                           writes=["mixed"], dma_key="xtbxt")
                                else:
                                    combine_tile(Sx, gi, xt, x1b[0][:], h2[:], "mixed", "x1_0", "h2", xkey="xtbxt")
                                    Sx.add("sp", lambda e, xt=xt, gi=gi: e.dma_start(out=xres[gi * 128:(gi + 1) * 128, :], in_=xt),
                                           reads=["mixed"], writes=[("xresS", tt)], dma_key=f"xresA{tt}")
                                rstd_ops(Sx, xt, D, sm[:, 0:1], sm[:, 1:2], junk[:], ["mixed"], "rs1")
                                Sx.add("dve", lambda e, xt=xt: e.tensor_scalar(out=hb[:], in0=xt, scalar1=sm[:, 1:2], scalar2=None, op0=ALU.mult),
                                       reads=["mixed", "rs1"], writes=["hb"])
                                pT = bkb(0, 8)
                                for c in range(8):
                                    Sx.add("pe", lambda e, c=c: e.transpose(out=pT[:, c, :], in_=hb[:, c * 128:(c + 1) * 128], identity=ident_b),
                                           reads=["hb", "cB"], writes=["B0"])
                                Sx.add("act", lambda e: e.copy(out=hT[:], in_=pT), reads=["B0"], writes=["hT"])
                                for c in range(8):
                                    Sx.add("pe", lambda e, c=c: e.matmul(bkf(1)[:, 0:416], lhsT=hT[:, c, :], rhs=win_b[:, c, 0:416], start=(c == 0), stop=(c == 7)),
                                           reads=["hT", "wts"], writes=["B1"])
                                for c in range(8):
                                    Sx.add("pe", lambda e, c=c: e.matmul(bkf(1)[:, 416:424], lhsT=hT[:, c, :], rhs=win_b[:, c, 1952:1960], start=(c == 0), stop=(c == 7)),
                                           reads=["hT", "wts"], writes=["B1"])
                                for c in range(8):
                                    Sx.add("pe", lambda e, c=c: e.matmul(bkf(2)[:, 0:512], lhsT=hT[:, c, :], rhs=win_b[:, c, 1440:1952], start=(c == 0), stop=(c == 7)),
                                           reads=["hT", "wts"], writes=["B2"])
                                for p in range(4):
                                    for c in range(8):
                                        Sx.add("pe", lambda e, c=c, p=p: e.matmul(bkf(3)[:, p * 128:(p + 1) * 128], lhsT=win_b[:, c, 416 + p * 128:416 + (p + 1) * 128],
                                                                                  rhs=hT[:, c, :], start=(c == 0), stop=(c == 7)),
                                               reads=["hT", "wts"], writes=["B3"])
                                for p in range(4):
                                    for c in range(8):
                                        Sx.add("pe", lambda e, c=c, p=p: e.matmul(bkf(4)[:, p * 128:(p + 1) * 128], lhsT=win_b[:, c, 928 + p * 128:928 + (p + 1) * 128],
                                                                                  rhs=hT[:, c, :], start=(c == 0), stop=(c == 7)),
                                               reads=["hT", "wts"], writes=["B4"])
                                Sx.add("act", lambda e, tt=tt: e.copy(out=fqT[:, :, tt * 128:(tt + 1) * 128], in_=bkf(3).rearrange("p (a b) -> p a b", a=4)),
                                       reads=["B3"], writes=["fqT"])
                                Sx.add("act", lambda e, ti=ti: e.copy(out=fkT[:, :, ti * 128:(ti + 1) * 128], in_=bkf(4).rearrange("p (a b) -> p a b", a=4)),
                                       reads=["B4"], writes=["fkT"])
                                Sx.add("act", lambda e: e.copy(out=tm1[:], in_=bkf(1)[:, 0:424]), reads=["B1"], writes=["tm1"])
                                rstd_ops(Sx, tm1[:, 0:256], 256, sm[:, 2:3], sm[:, 3:4], junk[:, 0:256], ["tm1"], "rsq")
                                rstd_ops(Sx, tm1[:, 256:384], 128, sm[:, 4:5], sm[:, 5:6], junk[:, 256:384], ["tm1"], "rskv")
                                Sx.add("dve", lambda e: e.tensor_scalar(out=qkn[:, 0:256], in0=tm1[:, 0:256], scalar1=sm[:, 3:4], scalar2=None, op0=ALU.mult),
                                       reads=["tm1", "rsq"], writes=["qkn"])
                                Sx.add("dve", lambda e: e.tensor_scalar(out=qkn[:, 256:384], in0=tm1[:, 256:384], scalar1=sm[:, 5:6], scalar2=None, op0=ALU.mult),
                                       reads=["tm1", "rskv"], writes=["qkn"])
                                pT2 = bkb(0, 8)
                                for c in range(3):
                                    Sx.add("pe", lambda e, c=c: e.transpose(out=pT2[:, c, :], in_=qkn[:, c * 128:(c + 1) * 128], identity=ident_b),
                                           reads=["qkn", "cB"], writes=["B0"])
                                Sx.add("act", lambda e: e.copy(out=qkT[:], in_=pT2[:, 0:3, :]), reads=["B0"], writes=["qkT"])
                                for hh in range(2):
                                    for c in range(2):
                                        Sx.add("pe", lambda e, c=c, hh=hh: e.matmul(bkf(5 + hh)[:, 0:384], lhsT=qkT[:, c, :], rhs=wuq_b[:, c, hh * 384:(hh + 1) * 384],
                                                                                    start=(c == 0), stop=(c == 1)),
                                               reads=["qkT", "wts"], writes=[f"B{5 + hh}"])
                                cosb = cos_t[:, gi, :].unsqueeze(1).broadcast_to([128, 4, 16])
                                sinb = sin_t[:, gi, :].unsqueeze(1).broadcast_to([128, 4, 16])
                                for hh in range(2):
                                    qv = bkf(5 + hh)[:, 0:384].rearrange("p (a b) -> p a b", a=4)
                                    bt = f"B{5 + hh}"
                                    Sx.add("act", lambda e, qv=qv, hh=hh: e.copy(out=q_tm[:, hh * 4:(hh + 1) * 4, 0:64], in_=qv[:, :, 0:64]),
                                           reads=[bt], writes=["q_tm"])
                                    Sx.add("dve", lambda e, qv=qv, cosb=cosb: e.tensor_tensor(out=rt[:, 0], in0=qv[:, :, 64:80], in1=cosb, op=ALU.mult), reads=[bt, "cos"], writes=["rt0"])
                                    Sx.add("dve", lambda e, qv=qv, sinb=sinb: e.tensor_tensor(out=rt[:, 1], in0=qv[:, :, 80:96], in1=sinb, op=ALU.mult), reads=[bt, "sin"], writes=["rt1"])
                                    Sx.add("dve", lambda e, qv=qv, sinb=sinb: e.tensor_tensor(out=rt[:, 2], in0=qv[:, :, 64:80], in1=sinb, op=ALU.mult), reads=[bt, "sin"], writes=["rt2"])
                                    Sx.add("dve", lambda e, qv=qv, cosb=cosb: e.tensor_tensor(out=rt[:, 3], in0=qv[:, :, 80:96], in1=cosb, op=ALU.mult), reads=[bt, "cos"], writes=["rt3"])
                                    Sx.add("dve", lambda e, hh=hh: e.tensor_tensor(out=q_tm[:, hh * 4:(hh + 1) * 4, 64:80], in0=rt[:, 0], in1=rt[:, 1], op=ALU.subtract),
                                           reads=["rt0", "rt1"], writes=["q_tm"])
                                    Sx.add("dve", lambda e, hh=hh: e.tensor_tensor(out=q_tm[:, hh * 4:(hh + 1) * 4, 80:96], in0=rt[:, 2], in1=rt[:, 3], op=ALU.add),
                                           reads=["rt2", "rt3"], writes=["q_tm"])
                                c1 = cos_t[:, gi, :]
                                s1 = sin_t[:, gi, :]
                                Sx.add("dve", lambda e, c1=c1: e.tensor_tensor(out=rt[:, 0, 0], in0=tm1[:, 384:400], in1=c1, op=ALU.mult), reads=["tm1", "cos", "q_tm"], writes=["rt0"])
                                Sx.add("dve", lambda e, s1=s1: e.tensor_tensor(out=rt[:, 1, 0], in0=tm1[:, 400:416], in1=s1, op=ALU.mult), reads=["tm1", "sin", "q_tm"], writes=["rt1"])
                                Sx.add("dve", lambda e, s1=s1: e.tensor_tensor(out=rt[:, 2, 0], in0=tm1[:, 384:400], in1=s1, op=ALU.mult), reads=["tm1", "sin", "q_tm"], writes=["rt2"])
                                Sx.add("dve", lambda e, c1=c1: e.tensor_tensor(out=rt[:, 3, 0], in0=tm1[:, 400:416], in1=c1, op=ALU.mult), reads=["tm1", "cos", "q_tm"], writes=["rt3"])
                                Sx.add("dve", lambda e: e.tensor_tensor(out=kpe[:, 0:16], in0=rt[:, 0, 0], in1=rt[:, 1, 0], op=ALU.subtract), reads=["rt0", "rt1"], writes=["kpe"])
                                Sx.add("dve", lambda e: e.tensor_tensor(out=kpe[:, 16:32], in0=rt[:, 2, 0], in1=rt[:, 3, 0], op=ALU.add), reads=["rt2", "rt3"], writes=["kpe"])
                                Sx.add("dve", lambda e: e.tensor_copy(out=k_tm[:, :, 64:96], in_=kpe[:].unsqueeze(1).broadcast_to([128, H, 32])), reads=["kpe"], writes=["k_tm"])
                                pTq = bkb(7, 8)
                                for h in range(H):
                                    Sx.add("pe", lambda e, h=h: e.transpose(out=pTq[0:96, h, :], in_=q_tm[:, h, :], identity=ident_b),
                                           reads=["q_tm", "cB"], writes=["B7"])
                                Sx.add("act", lambda e, tt=tt: e.copy(out=QT[0:96, :, tt * 128:(tt + 1) * 128], in_=pTq[0:96, :, :]), reads=["B7"], writes=["QT"])
                                for hh in range(2):
                                    Sx.add("pe", lambda e, hh=hh: e.matmul(bkf(5 + hh)[:, 0:512], lhsT=qkT[:, 2, :], rhs=wukv_b[:, hh * 512:(hh + 1) * 512], start=True, stop=True),
                                           reads=["qkT", "wts"], writes=[f"B{5 + hh}"])
                                for hh in range(2):
                                    kvv = bkf(5 + hh).rearrange("p (a b) -> p a b", a=4)
                                    bt = f"B{5 + hh}"
                                    Sx.add("act", lambda e, kvv=kvv, hh=hh: e.copy(out=k_tm[:, hh * 4:(hh + 1) * 4, 0:64], in_=kvv[:, :, 0:64]), reads=[bt], writes=["k_tm"])
                                    Sx.add("dve", lambda e, kvv=kvv, hh=hh, ti=ti: e.tensor_copy(out=Vm[:, ti, hh * 4:(hh + 1) * 4, 0:64], in_=kvv[:, :, 64:128]),
                                           reads=[bt], writes=["Vm"])
                                pTk = bkb(7, 8)
                                for h in range(H):
                                    Sx.add("pe", lambda e, h=h: e.transpose(out=pTk[0:96, h, :], in_=k_tm[:, h, :], identity=ident_b),
                                           reads=["k_tm", "cB"], writes=["B7"])
                                Sx.add("act", lambda e, ti=ti: e.copy(out=KT[0:96, :, ti * 128:(ti + 1) * 128], in_=pTk[0:96, :, :]), reads=["B7"], writes=["KT"])
                                Sx.add("dve", lambda e: e.tensor_tensor(out=lsp[:, 0:8], in0=tm1[:, 416:424], in1=bfg_bc[:], op=ALU.add), reads=["tm1", "bfg"], writes=["lsp0"])
                                Sx.add("act", lambda e: e.activation(out=lsp[:, 8:16], in_=lsp[:, 0:8], func=AF.Exp, scale=-1.0), reads=["lsp0"], writes=["lsp1"])
                                Sx.add("act", lambda e: e.activation(out=lsp[:, 16:24], in_=lsp[:, 8:16], func=AF.Ln, bias=1.0, scale=1.0), reads=["lsp1"], writes=["lsp2"])
                                Sx.add("pe", lambda e: e.matmul(bkf(7)[:, 0:8], lhsT=utri_f, rhs=lsp[:, 16:24], start=True, stop=True), reads=["lsp2", "cF"], writes=["B7"])
                                Sx.add("pe", lambda e: e.matmul(bkf(7)[:, 8:16], lhsT=ones_f, rhs=lsp[:, 16:24], start=True, stop=True), reads=["lsp2", "cF"], writes=["B7"])
                                Sx.add("dve", lambda e, ti=ti: e.tensor_tensor(out=Lc[:, ti + 1, :], in0=bkf(7)[:, 8:16], in1=Lc[:, ti, :], op=ALU.add), reads=["B7", "Lc"], writes=["Lc"])
                                Sx.add("dve", lambda e, ti=ti: e.tensor_tensor(out=lsp[:, 24:32], in0=bkf(7)[:, 0:8], in1=Lc[:, ti, :], op=ALU.add), reads=["B7", "Lc"], writes=["lsp3"])
                                Sx.add("dve", lambda e, ti=ti: e.tensor_tensor(out=lsp[:, 24:32], in0=lsp[:, 24:32], in1=Lc[:, ti + 1, :], op=ALU.subtract), reads=["lsp3", "Lc"], writes=["lsp3"])
                                Sx.add("act", lambda e: e.activation(out=lsp[:, 0:8], in_=lsp[:, 24:32], func=AF.Exp), reads=["lsp3", "lsp0", "lsp1"], writes=["lsp0"])
                                Sx.add("dve", lambda e, ti=ti: e.tensor_tensor(out=Vf[:, ti, :, 0:64], in0=bkf(2).rearrange("p (a b) -> p a b", a=H),
                                                                               in1=lsp[:, 0:8].unsqueeze(2).broadcast_to([128, H, 64]), op=ALU.mult),
                                       reads=["B2", "lsp0"], writes=["Vf"])
                                Sx.add("dve", lambda e, ti=ti: e.tensor_copy(out=Vf[:, ti, :, 64:65], in_=lsp[:, 0:8].unsqueeze(2)), reads=["lsp0"], writes=["Vf"])

                            chk("A")
                            q0t = ch * 4
                            rot = [0, 0, 0]

                            steps = []

                            def attend(kind, h, qa, qn_):
                                ob = 4 + (rot[1] % 2)
                                rot[1] += 1
                                obt = f"B{ob}"
                                jlast = (qa + qn_) // 128 - 1
                                iend = jlast
                                for j in range(jlast + 1):
                                    front = []
                                    qlo = max(qa, j * 128)
                                    n = qa + qn_ - qlo
                                    cq = qlo - q0t * 128
                                    sb = rot[0] % 4
                                    rot[0] += 1
                                    sbt = f"B{sb}"
                                    if kind == "mla":
                                        lhsT = KT[0:96, h, j * 128:(j + 1) * 128]
                                        rhs = QT[0:96, h, cq:cq + n]
                                        rtok = ["KT", "QT"]
                                    else:
                                        pb = (h % 2) * 64
                                        lhsT = fkT[pb:pb + 64, h // 2, j * 128:(j + 1) * 128]
                                        rhs = fqT[pb:pb + 64, h // 2, cq:cq + n]
                                        rtok = ["fkT", "fqT"]
                                    front.append(("pe", lambda e, sb=sb, n=n, lhsT=lhsT, rhs=rhs: e.matmul(bkf(sb)[:, 0:n], lhsT=lhsT, rhs=rhs, start=True, stop=True),
                                                  rtok, [sbt]))
                                    pbuf = Pb[sb]
                                    if kind == "mla":
                                        front.append(("act", lambda e, sb=sb, n=n, pbuf=pbuf: e.activation(out=pbuf[:, 0:n], in_=bkf(sb)[:, 0:n], func=AF.Exp, scale=96 ** -0.5),
                                                      [sbt], [f"P{sb}"]))
                                    else:
                                        dv, dtok = dbias[(iend, j)]
                                        bap = dv[:, h:h + 1]
                                        front.append(("act", lambda e, sb=sb, n=n, pbuf=pbuf, bap=bap: e.activation(
                                            out=pbuf[:, 0:n], in_=bkf(sb)[:, 0:n], func=AF.Exp, scale=0.125, bias=bap),
                                            [sbt, dtok], [f"P{sb}"]))
                                    if j * 128 >= qa:
                                        mk = mask_mla if kind == "mla" else mask_fox
                                        front.append(("pool", lambda e, pbuf=pbuf, mk=mk: e.tensor_tensor(out=pbuf[:, 0:128], in0=pbuf[:, 0:128], in1=mk, op=ALU.mult),
                                                      [f"P{sb}", "cB"], [f"P{sb}"]))
                                    vv = (Vm if kind == "mla" else Vf)[:, j, h, :]
                                    co = qlo - qa
                                    pv = [("pe", lambda e, ob=ob, co=co, n=n, vv=vv, pbuf=pbuf, j=j, jlast=jlast: e.matmul(bkf(ob)[0:65, co:co + n], lhsT=vv, rhs=pbuf[:, 0:n],
                                                                                                                   start=(j == 0), stop=(j == jlast)),
                                           [f"P{sb}", "Vm" if kind == "mla" else "Vf", "Vm1"], [obt])]
                                    steps.append({"front": front, "pv": pv, "epi": None})
                                epi = []
                                osb = Osb[ob - 4]
                                ost = f"Osb{ob - 4}"
                                epi.append(("dve", lambda e, ob=ob, osb=osb: e.tensor_copy(out=osb[:, 0:qn_], in_=bkf(ob)[0:65, 0:qn_]), [obt], [ost]))
                                nq = qn_ // 128
                                ptr = bkf(6)[:, 0:nq * 65].rearrange("p (a b) -> p a b", a=nq)
                                for a_ in range(nq):
                                    epi.append(("pe", lambda e, a_=a_, osb=osb, ptr=ptr: e.transpose(out=ptr[:, a_, :], in_=osb[:, a_ * 128:(a_ + 1) * 128], identity=ident_f[0:65, 0:65]),
                                                [ost, "cF"], ["B6"]))
                                epi.append(("dve", lambda e, ptr=ptr, nq=nq: e.reciprocal(out=rden[:, 0:nq], in_=ptr[:, :, 64]), ["B6"], ["rden"]))
                                col = (0 if kind == "mla" else 512) + h * 64
                                for a_ in range(nq):
                                    tloc = (qa // 128 - q0t) + a_
                                    epi.append(("dve", lambda e, a_=a_, ptr=ptr, tloc=tloc, col=col: e.tensor_scalar(out=mixed[:, tloc, col:col + 64], in0=ptr[:, a_, 0:64],
                                                                                                                 scalar1=rden[:, a_:a_ + 1], scalar2=None, op0=ALU.mult),
                                                ["B6", "rden"], ["mixed"]))
                                steps[-1]["epi"] = epi

                            def run_steps(LA=3, EPI_DELAY=1):
                                def emit(ops):
                                    for (eng_, fn_, r_, w_) in ops:
                                        Sx.add(eng_, fn_, reads=r_, writes=w_)
                                pend = []
                                ns = len(steps)
                                for k in range(ns + LA + EPI_DELAY + 1):
                                    if k < ns:
                                        emit(steps[k]["front"])
                                    kp = k - LA
                                    if 0 <= kp < ns:
                                        emit(steps[kp]["pv"])
                                        if steps[kp]["epi"] is not None:
                                            pend.append((k + EPI_DELAY, steps[kp]["epi"]))
                                    while pend and pend[0][0] <= k:
                                        emit(pend.pop(0)[1])
                                for _, ops in pend:
                                    emit(ops)

                            dbias = {}
                            kdb = 0
                            for sc in range(2):
                                iend = q0t + 2 * sc + 1
                                for j in range(iend + 1):
                                    dv = dbt[:, kdb, :]
                                    kdb += 1
                                    dbias[(iend, j)] = (dv, ("dbias", kdb))
                                    Sx.add("dve", lambda e, dv=dv, j=j, iend=iend: e.tensor_tensor(out=dv, in0=Lc[:, j + 1, :], in1=Lc[:, iend + 1, :], op=ALU.subtract),
                                           reads=["Lc"], writes=[("dbias", kdb)])
                            for h in range(H):
                                attend("mla", h, q0t * 128, 512)
                                for sc in range(2):
                                    attend("fox", h, q0t * 128 + sc * 256, 256)
                            run_steps()

                            chk("att")
                            def phaseB(tt):
                                ti = ch * 4 + tt
                                gi = sq * NTS + ti
                                mx = mixed[:, tt, :]
                                x1 = x1b[tt % 2]
                                x1t = f"x1_{tt % 2}"
                                if l == 0:
                                    Sx.add("sp", lambda e, x1=x1, gi=gi: e.dma_start(out=x1[:], in_=x_d[gi * 128:(gi + 1) * 128, :]), writes=[x1t], dma_key=x1t)
                                else:
                                    Sx.add("sp", lambda e, x1=x1, gi=gi: e.dma_start(out=x1[:], in_=xres[gi * 128:(gi + 1) * 128, :]), reads=[("xresS", tt)], writes=[x1t], dma_key=x1t)
                                rstd_ops(Sx, mx[:, 0:512], 512, sm[:, 8:9], sm[:, 9:10], junk[:, 0:512], ["mixed"], "rsm")
                                rstd_ops(Sx, mx[:, 512:1024], 512, sm[:, 10:11], sm[:, 11:12], junk[:, 512:1024], ["mixed"], "rsf")
                                pTm = bkb(0, 8)
                                for c in range(8):
                                    Sx.add("pe", lambda e, c=c, mx=mx: e.transpose(out=pTm[:, c, :], in_=mx[:, c * 128:(c + 1) * 128], identity=ident_b),
                                           reads=["mixed", "cB"], writes=["B0"])
                                Sx.add("act", lambda e: e.copy(out=mT[:], in_=pTm), reads=["B0"], writes=["hT"])
                                for grp in range(2):
                                    for nh in range(2):
                                        bk = 1 + grp * 2 + nh
                                        for c in range(4):
                                            Sx.add("pe", lambda e, bk=bk, c=c, grp=grp, nh=nh: e.matmul(bkf(bk)[:, 0:512], lhsT=mT[:, grp * 4 + c, :],
                                                                                                         rhs=wout_b[:, grp * 4 + c, nh * 512:(nh + 1) * 512],
                                                                                                         start=(c == 0), stop=(c == 3)),
                                                   reads=["hT", "wts"], writes=[f"B{bk}"])
                                for nh in range(2):
                                    Sx.add("dve", lambda e, x1=x1, nh=nh: e.scalar_tensor_tensor(out=x1[:, nh * 512:(nh + 1) * 512], in0=bkf(1 + nh), scalar=sm[:, 9:10],
                                                                                          in1=x1[:, nh * 512:(nh + 1) * 512], op0=ALU.mult, op1=ALU.add),
                                           reads=[f"B{1 + nh}", "rsm", x1t], writes=[x1t])
                                    Sx.add("dve", lambda e, x1=x1, nh=nh: e.scalar_tensor_tensor(out=x1[:, nh * 512:(nh + 1) * 512], in0=bkf(3 + nh), scalar=sm[:, 11:12],
                                                                                          in1=x1[:, nh * 512:(nh + 1) * 512], op0=ALU.mult, op1=ALU.add),
                                           reads=[f"B{3 + nh}", "rsf", x1t], writes=[x1t])
                                Sx.add("sp", lambda e, x1=x1, gi=gi: e.dma_start(out=xres[gi * 128:(gi + 1) * 128, :], in_=x1[:]), reads=[x1t], writes=[("xres", gi)], dma_key=f"xres{tt % 2}")

                            def phaseC(tt):
                                ti = ch * 4 + tt
                                gi = sq * NTS + ti
                                x1 = x1b[tt % 2]
                                x1t = f"x1_{tt % 2}"
                                rstd_ops(Sx, x1[:], D, sm[:, 12:13], sm[:, 13:14], junk[:], [x1t], "rs2")
                                Sx.add("dve", lambda e, x1=x1: e.scalar_tensor_tensor(out=h2[:], in0=x1[:], scalar=sm[:, 13:14], in1=gffn_bc[:], op0=ALU.mult, op1=ALU.mult),
                                       reads=[x1t, "rs2", "gffn"], writes=["h2"])
                                Sx.add("act", lambda e: e.copy(out=h2b[:], in_=h2[:]), reads=["h2"], writes=["hb"])
                                for c in range(8):
                                    bk = 5 + c // 4
                                    Sx.add("pe", lambda e, c=c, bk=bk: e.transpose(out=bkf(bk)[:, (c % 4) * 128:(c % 4 + 1) * 128], in_=h2[:, c * 128:(c + 1) * 128], identity=ident_f),
                                           reads=["h2", "cF"], writes=[f"B{bk}"])
                                Sx.add("act", lambda e: e.copy(out=h2T[:, 0:4, :], in_=bkf(5).rearrange("p (a b) -> p a b", a=4)), reads=["B5"], writes=["h2Ta"])
                                Sx.add("dve", lambda e: e.tensor_copy(out=h2T[:, 4:8, :], in_=bkf(6).rearrange("p (a b) -> p a b", a=4)), reads=["B6"], writes=["h2Tb"])
                                for c in range(8):
                                    Sx.add("pe", lambda e, c=c: e.matmul(bkf(7)[:, 0:G + E], lhsT=h2T[:, c, :], rhs=wr_f[:, c, :], start=(c == 0), stop=(c == 7)),
                                           reads=["h2Ta", "h2Tb", "wr"], writes=["B7"])
                                Sx.add("dve", lambda e: e.tensor_tensor(out=lg[:], in0=bkf(7)[:, 0:G + E], in1=br_bc[:], op=ALU.add), reads=["B7", "br"], writes=["lg"])
                                Sx.add("dve", lambda e: e.tensor_reduce(out=rs[:, 0:1], in_=lg[:, 0:G], axis=mybir.AxisListType.X, op=ALU.max), reads=["lg"], writes=["r_gmax"])
                                Sx.add("dve", lambda e: e.tensor_scalar(out=rs[:, 1:2], in0=rs[:, 0:1], scalar1=-1.0, scalar2=None, op0=ALU.mult), reads=["r_gmax"], writes=["r_ngmax"])
                                Sx.add("act", lambda e: e.activation(out=rs[:, 146:150], in_=lg[:, 0:G], func=AF.Exp, bias=rs[:, 1:2], scale=1.0, accum_out=rs[:, 2:3]),
                                       reads=["lg", "r_ngmax"], writes=["r_sg", "r_j4"])
                                Sx.add("dve", lambda e: e.reciprocal(out=rs[:, 3:4], in_=rs[:, 2:3]), reads=["r_sg"], writes=["r_gval"])
                                Sx.add("dve", lambda e: e.tensor_scalar(out=rs[:, 4:8], in0=lg[:, 0:G], scalar1=rs[:, 0:1], scalar2=None, op0=ALU.is_equal), reads=["lg", "r_gmax"], writes=["r_ohg"])
                                Sx.add("dve", lambda e: e.tensor_scalar(out=rs[:, 8:16], in0=lg[:, G:G + 8], scalar1=rs[:, 4:5], scalar2=None, op0=ALU.mult), reads=["lg", "r_ohg"], writes=["r_esel"])
                                for g in range(1, G):
                                    Sx.add("dve", lambda e, g=g: e.scalar_tensor_tensor(out=rs[:, 8:16], in0=lg[:, G + 8 * g:G + 8 * g + 8], scalar=rs[:, 4 + g:5 + g], in1=rs[:, 8:16],
                                                                                       op0=ALU.mult, op1=ALU.add), reads=["lg", "r_ohg", "r_esel"], writes=["r_esel"])
                                Sx.add("dve", lambda e: e.max(out=rs[:, 16:24], in_=rs[:, 8:16]), reads=["r_esel"], writes=["r_top"])
                                Sx.add("dve", lambda e: e.tensor_tensor(out=rs[:, 24:25], in0=rs[:, 17:18], in1=rs[:, 16:17], op=ALU.subtract), reads=["r_top"], writes=["r_d"])
                                Sx.add("act", lambda e: e.activation(out=rs[:, 25:26], in_=rs[:, 24:25], func=AF.Exp), reads=["r_d"], writes=["r_e2"])
                                Sx.add("dve", lambda e: e.tensor_scalar(out=rs[:, 26:27], in0=rs[:, 25:26], scalar1=1.0, scalar2=None, op0=ALU.add), reads=["r_e2"], writes=["r_den"])
                                Sx.add("dve", lambda e: e.reciprocal(out=rs[:, 27:28], in_=rs[:, 26:27]), reads=["r_den"], writes=["r_rden"])
                                Sx.add("dve", lambda e, gi=gi: e.tensor_tensor(out=gates[:, gi, 0:1], in0=rs[:, 3:4], in1=rs[:, 27:28], op=ALU.mult),
                                       reads=["r_gval", "r_rden", ("gate", gi)], writes=[("gate", gi)])
                                Sx.add("dve", lambda e, gi=gi: e.tensor_tensor(out=gates[:, gi, 1:2], in0=gates[:, gi, 0:1], in1=rs[:, 25:26], op=ALU.mult),
                                       reads=["r_e2", ("gate", gi)], writes=[("gate", gi)])
                                Sx.add("dve", lambda e: e.tensor_scalar(out=rs[:, 32:40], in0=rs[:, 8:16], scalar1=rs[:, 16:17], scalar2=None, op0=ALU.is_equal), reads=["r_esel", "r_top"], writes=["r_oh1"])
                                Sx.add("dve", lambda e: e.tensor_scalar(out=rs[:, 40:48], in0=rs[:, 8:16], scalar1=rs[:, 17:18], scalar2=None, op0=ALU.is_equal), reads=["r_esel", "r_top"], writes=["r_oh2"])
                                for g in range(G):
                                    Sx.add("dve", lambda e, g=g: e.tensor_scalar(out=rs[:, 48 + 8 * g:56 + 8 * g], in0=rs[:, 32:40], scalar1=rs[:, 4 + g:5 + g], scalar2=None, op0=ALU.mult),
                                           reads=["r_oh1", "r_ohg"], writes=["r_A1"])
                                    Sx.add("dve", lambda e, g=g: e.tensor_scalar(out=rs[:, 80 + 8 * g:88 + 8 * g], in0=rs[:, 40:48], scalar1=rs[:, 4 + g:5 + g], scalar2=None, op0=ALU.mult),
                                           reads=["r_oh2", "r_ohg"], writes=["r_A2"])
                                Sx.add("dve", lambda e: e.tensor_tensor(out=asum_b[:], in0=rs[:, 48:80], in1=rs[:, 80:112], op=ALU.add), reads=["r_A1", "r_A2"], writes=["asum"])
                                Sx.add("pe", lambda e: e.matmul(bkf(7)[:, 64:96], lhsT=stri_b, rhs=asum_b[:], start=True, stop=True), reads=["asum", "cB"], writes=["B7"])
                                Sx.add("pe", lambda e: e.matmul(bkf(7)[:, 96:128], lhsT=ones_b, rhs=asum_b[:], start=True, stop=True), reads=["asum", "cB"], writes=["B7"])
                                Sx.add("dve", lambda e: e.tensor_tensor(out=rs[:, 112:144], in0=bkf(7)[:, 64:96], in1=ccarry[:], op=ALU.add), reads=["B7", "ccarry"], writes=["r_slot"])
                                Sx.add("dve", lambda e: e.tensor_tensor(out=ccarry[:], in0=bkf(7)[:, 96:128], in1=ccarry[:], op=ALU.add), reads=["B7", "ccarry", "r_slot"], writes=["ccarry"])
                                Sx.add("dve", lambda e: e.tensor_scalar(out=rs[:, 112:144], in0=rs[:, 112:144], scalar1=float(CAP - 1), scalar2=None, op0=ALU.min), reads=["r_slot"], writes=["r_slot"])
                                Sx.add("dve", lambda e: e.tensor_tensor(out=rs[:, 112:144], in0=rs[:, 112:144], in1=ebase, op=ALU.add), reads=["r_slot", "cF"], writes=["r_slot"])
                                Sx.add("dve", lambda e: e.scalar_tensor_tensor(out=junk[:, 0:32], in0=rs[:, 48:80], scalar=1.0, in1=rs[:, 112:144], op0=ALU.mult, op1=ALU.mult, accum_out=rs[:, 144:145]),
                                       reads=["r_A1", "r_slot"], writes=["r_d1", "junk"])
                                Sx.add("dve", lambda e: e.scalar_tensor_tensor(out=junk[:, 32:64], in0=rs[:, 80:112], scalar=1.0, in1=rs[:, 112:144], op0=ALU.mult, op1=ALU.mult, accum_out=rs[:, 145:146]),
                                       reads=["r_A2", "r_slot"], writes=["r_d2", "junk"])
                                Sx.add("dve", lambda e, gi=gi: e.tensor_copy(out=dest_i[:, gi, :], in_=rs[:, 144:146]), reads=["r_d1", "r_d2", ("dest", gi)], writes=[("dest", gi)])
                                for k in range(2):
                                    Sx.add("pool", lambda e, gi=gi, k=k: e.indirect_dma_start(out=xbuf, out_offset=bass.IndirectOffsetOnAxis(ap=dest_i[:, gi, k:k + 1], axis=0),
                                                                                             in_=h2b[:], in_offset=None, bounds_check=Sx.reg(e, NSLOT - 1), oob_is_err=False),
                                           reads=["hb", ("dest", gi)], writes=[("xbuf", gi, k)], dma_key="xscat")

                            phaseB(0)
                            phaseB(1)
                            phaseC(0)
                            chk("B")
                            phaseB(2)
                            phaseC(1)
                            phaseB(3)
                            phaseC(2)
                            phaseC(3)
                except StopBuild:
                    stopped = True
                Sx.emit(f"attn{l}")
            if stop_after == ("attn", l) or stopped:
                return nc

            with ExitStack() as es:
                def T(name, shape, dt):
                    return es.enter_context(nc.sbuf_tensor(uname(name), list(shape), dt))
                Sx = Sched(nc)
                wg_b = [T(f"wg_b{i}", [128, 8, DE], BF16) for i in range(2)]
                wu_b = [T(f"wu_b{i}", [128, 8, DE], BF16) for i in range(2)]
                wd_b = [T(f"wd_b{i}", [128, 4, D], BF16) for i in range(2)]
                xb = [T(f"xb{i}", [128, D], BF16) for i in range(2)]
                xT = T("xT", [128, 8, 128], BF16)
                hmT = T("hmT", [128, 4, 128], BF16)
                ysb = [T(f"ysb{i}", [128, D], F32) for i in range(2)]
                sg = [T(f"sg{i}", [128, DE], F32) for i in range(2)]
                hm = [T(f"hm{i}", [128, DE], BF16) for i in range(2)]
                blocks = []
                for ex in range(E):
                    for jb in range(NBLK):
                        blocks.append((ex, ex % 2, jb, ex * CAP + jb * 128))

                def wload(ex):
                    sl = ex % 2
                    for half in range(2):
                        Sx.add("pool", lambda e, ex=ex, sl=sl, half=half: e.dma_start(
                            out=wg_b[sl][:, half * 4:(half + 1) * 4, :], in_=w_gate[l, ex, half * 512:(half + 1) * 512, :].rearrange("(c p) n -> p c n", p=128)),
                            writes=[f"wg{sl}_{half}"], dma_key=f"wg{sl}")
                        Sx.add("pool", lambda e, ex=ex, sl=sl, half=half: e.dma_start(
                            out=wu_b[sl][:, half * 4:(half + 1) * 4, :], in_=w_up[l, ex, half * 512:(half + 1) * 512, :].rearrange("(c p) n -> p c n", p=128)),
                            writes=[f"wu{sl}_{half}"], dma_key=f"wu{sl}")
                        Sx.add("pool", lambda e, ex=ex, sl=sl, half=half: e.dma_start(
                            out=wd_b[sl][:, half * 2:(half + 1) * 2, :], in_=w_down[l, ex, half * 256:(half + 1) * 256, :].rearrange("(c p) n -> p c n", p=128)),
                            writes=[f"wd{sl}_{half}"], dma_key=f"wd{sl}")

                def s_load(bi_):
                    ex, sl, jb, r0 = blocks[bi_]
                    xs = bi_ % 2
                    Sx.add("sp", lambda e, r0=r0, xs=xs: e.dma_start(out=xb[xs][:], in_=xbuf[r0:r0 + 128, :]), writes=[f"xb{xs}"], dma_key=f"xb{xs}")

                def s_T(bi_):
                    xs = bi_ % 2
                    pT = bkb(0, 8)
                    for c in range(8):
                        Sx.add("pe", lambda e, c=c, xs=xs, pT=pT: e.transpose(out=pT[:, c, :], in_=xb[xs][:, c * 128:(c + 1) * 128], identity=ident_b),
                               reads=[f"xb{xs}"], writes=["B0"])
                    Sx.add("act", lambda e, pT=pT: e.copy(out=xT[:], in_=pT), reads=["B0"], writes=["xT"])

                def s_GU(bi_):
                    ex, sl, jb, r0 = blocks[bi_]
                    k2 = bi_ % 2
                    for c in range(8):
                        Sx.add("pe", lambda e, c=c, sl=sl: e.matmul(bkf(1)[:, 0:512], lhsT=xT[:, c, :], rhs=wg_b[sl][:, c, :], start=(c == 0), stop=(c == 7)),
                               reads=["xT", f"wg{sl}_0", f"wg{sl}_1"], writes=["B1"])
                    for c in range(8):
                        Sx.add("pe", lambda e, c=c, sl=sl: e.matmul(bkf(2)[:, 0:512], lhsT=xT[:, c, :], rhs=wu_b[sl][:, c, :], start=(c == 0), stop=(c == 7)),
                               reads=["xT", f"wu{sl}_0", f"wu{sl}_1"], writes=["B2"])
                    Sx.add("act", lambda e, k2=k2: e.activation(out=sg[k2][:], in_=bkf(1), func=AF.Silu), reads=["B1"], writes=[f"sg{k2}"])
                    Sx.add("dve", lambda e, k2=k2: e.tensor_tensor(out=hm[k2][:], in0=sg[k2][:], in1=bkf(2), op=ALU.mult), reads=[f"sg{k2}", "B2"], writes=[f"hm{k2}"])

                def s_T2(bi_):
                    k2 = bi_ % 2
                    pT2 = bkb(3, 8)
                    for c in range(4):
                        Sx.add("pe", lambda e, c=c, k2=k2, pT2=pT2: e.transpose(out=pT2[:, c, :], in_=hm[k2][:, c * 128:(c + 1) * 128], identity=ident_b), reads=[f"hm{k2}"], writes=["B3"])
                    Sx.add("act", lambda e, pT2=pT2: e.copy(out=hmT[:], in_=pT2[:, 0:4, :]), reads=["B3"], writes=["hmT"])

                def s_D(bi_):
                    ex, sl, jb, r0 = blocks[bi_]
                    xs = bi_ % 2
                    for nh in range(2):
                        for c in range(4):
                            Sx.add("pe", lambda e, c=c, nh=nh, sl=sl: e.matmul(bkf(4 + nh)[:, 0:512], lhsT=hmT[:, c, :], rhs=wd_b[sl][:, c, nh * 512:(nh + 1) * 512],
                                                                               start=(c == 0), stop=(c == 3)),
                                   reads=["hmT", f"wd{sl}_0", f"wd{sl}_1"], writes=[f"B{4 + nh}"])
                    Sx.add("act", lambda e, xs=xs: e.copy(out=ysb[xs][:, 0:512], in_=bkf(4)), reads=["B4"], writes=[f"ysa{xs}"])
                    Sx.add("dve", lambda e, xs=xs: e.tensor_copy(out=ysb[xs][:, 512:1024], in_=bkf(5)), reads=["B5"], writes=[f"ysb{xs}"])
                    Sx.add("sp", lambda e, r0=r0, xs=xs: e.dma_start(out=ybuf[r0:r0 + 128, :], in_=ysb[xs][:]), reads=[f"ysa{xs}", f"ysb{xs}"], writes=[f"yo{xs}"], dma_key=f"yo{xs}")

                NB_ = len(blocks)
                wload(0)
                wload(1)
                s_load(0)
                s_load(1)
                s_T(0)
                s_GU(0)
                for bi_ in range(NB_):
                    nxt = bi_ + 1
                    if nxt < NB_:
                        if nxt + 1 < NB_:
                            s_load(nxt + 1)
                        s_T(nxt)
                    s_T2(bi_)
                    if nxt < NB_:
                        s_GU(nxt)
                    s_D(bi_)
                    ex, sl, jb, r0 = blocks[bi_]
                    if jb == NBLK - 1 and ex + 2 < E:
                        wload(ex + 2)
                Sx.emit(f"moe{l}")
            if stop_after == ("moe", l):
                return nc

        with ExitStack() as es:
            def T(name, shape, dt):
                return es.enter_context(nc.sbuf_tensor(uname(name), list(shape), dt))
            Sx = Sched(nc)
            gfin = T("gfin", [128, D], F32)
            xt2 = [T(f"xf{i}", [128, D], F32) for i in range(2)]
            y1f = [T(f"y1f{i}", [128, D], F32) for i in range(2)]
            y2f = [T(f"y2f{i}", [128, D], F32) for i in range(2)]
            junk = T("junkf", [128, D], F32)
            smf = [T(f"smf{i}", [128, 2], F32) for i in range(2)]
            Sx.add("sp", lambda e: e.dma_start(out=gfin[:], in_=fin_g.partition_broadcast(128)), writes=["gfin"], dma_key="gfin")
            for gi in range(NT):
                s_ = gi % 2
                key = f"f{s_}"
                combine_tile(Sx, gi, xt2[s_][:], y1f[s_][:], y2f[s_][:], key + "xt", key + "y1", key + "y2")
                rstd_ops(Sx, xt2[s_][:], D, smf[s_][:, 0:1], smf[s_][:, 1:2], junk[:], [key + "xt"], f"rsf{s_}")
                Sx.add("dve", lambda e, s_=s_: e.scalar_tensor_tensor(out=y1f[s_][:], in0=xt2[s_][:], scalar=smf[s_][:, 1:2], in1=gfin[:], op0=ALU.mult, op1=ALU.mult),
                       reads=[key + "xt", f"rsf{s_}", "gfin", key + "y1"], writes=[key + "y1"])
                Sx.add("sp", lambda e, s_=s_, gi=gi: e.dma_start(out=out_d[gi * 128:(gi + 1) * 128, :], in_=y1f[s_][:]), reads=[key + "y1"], writes=[key + "o"], dma_key=key + "o")
            Sx.emit("final")
    return nc


def host_consts(CAP):
    k = np.arange(128)[:, None]
    m = np.arange(128)[None, :]
    half = 16
    invf = (np.float32(10000.0) ** (-np.arange(half, dtype=np.float32) / np.float32(half))).astype(np.float32)
    cF = np.zeros((128, 128 * 3 + 48), np.float32)
    cF[:, 0:128] = np.eye(128)
    cF[:, 128:256] = (k <= m)
    cF[:, 256:384] = 1.0
    cF[:, 384:400] = invf[None, :]
    cF[:, 400:432] = (np.arange(E) * CAP)[None, :]
    cB = np.zeros((128, 640), np.float32)
    cB[:, 0:128] = np.eye(128)
    cB[:, 128:256] = (k < m)
    cB[:, 256:384] = 1.0
    cB[:, 384:512] = (k <= m)
    cB[:, 512:640] = (k // 64 <= m // 64)
    return cF, cB.astype(ml_dtypes.bfloat16)


def col_layout(v):
    Lh, n = v.shape
    return np.ascontiguousarray(v.reshape(Lh, n // 128, 128).transpose(0, 2, 1))


def make_in_maps(inp, n_cores, NB, S, CAP):
    f = lambda a: np.ascontiguousarray(np.asarray(a, dtype=np.float32))
    cF, cB = host_consts(CAP)
    shared = {
        "attn_g": col_layout(f(inp["attn_norm"])),
        "w_in": f(inp["w_in"]),
        "b_forget": f(inp["b_forget"]),
        "q_g": col_layout(f(inp["q_norm"])),
        "w_uq": f(inp["w_uq"]),
        "kv_g": col_layout(f(inp["kv_norm"])),
        "w_ukv": f(inp["w_ukv"]),
        "out_g": col_layout(np.concatenate([f(inp["mla_out_norm"]), f(inp["fox_out_norm"])], axis=1)),
        "w_out": f(inp["w_out"]),
        "ffn_g": f(inp["ffn_norm"]),
        "w_r": np.ascontiguousarray(np.concatenate([f(inp["w_router_group"]), f(inp["w_router_expert"])], axis=2)),
        "b_r": np.ascontiguousarray(np.concatenate([f(inp["b_router_group"]), f(inp["b_router_expert"])], axis=1)),
        "w_gate": f(inp["w_gate"]),
        "w_up": f(inp["w_up"]),
        "w_down": f(inp["w_down"]),
        "fin_g": f(inp["final_norm"]),
        "cF": cF,
        "cB": cB,
    }
    x = f(inp["x"])
    pos = np.asarray(inp["positions"]).astype(np.int32)
    maps = []
    for c in range(n_cores):
        xs = x[c * NB:(c + 1) * NB].reshape(NB * S, D)
        ps = pos[c * NB:(c + 1) * NB].reshape(NB * S // 128, 128).T
        m = dict(shared)
        m["x"] = np.ascontiguousarray(xs)
        m["pos"] = np.ascontiguousarray(ps)
        maps.append(m)
    return maps


CAP_FULL = 640


def kernel(**inputs):
    n_cores = 8
    B, S, _ = inputs["x"].shape
    NB = B // n_cores
    nc = build(NB, S, CAP_FULL)
    maps = make_in_maps(inputs, n_cores, NB, S, CAP_FULL)
    res = run_bass_kernel_spmd(nc, maps, core_ids=list(range(n_cores)))
    out = np.concatenate([r["out"].reshape(NB, S, D) for r in res.results], axis=0)
    return out.astype(np.float32)
```
